# Optimizing a Trainium2 kernel written in Bass

```python
import jax, jax.numpy as jnp
from jax import lax
import numpy as np

D_MODEL = 2048
BATCH = 1
SEQ = 8192
DEPTH = 1

CHUNK = 64
CONV_WIDTH = 1024
CONV_K = 3
SGU_WIDTH = 1024
SGU_HEADS = 8
SGU_HEAD_DIM = SGU_WIDTH // SGU_HEADS
SGU_BLOCK = 128
N_BRANCH = 2
IN_COLS = 3 * CONV_WIDTH + 2 * SGU_WIDTH + N_BRANCH * D_MODEL
N_GROUPS = 4
EXPERTS_PER_GROUP = 4
N_EXPERTS = N_GROUPS * EXPERTS_PER_GROUP
TOP_K_INNER = 2
D_EXPERT = D_MODEL // 4
EPS = 1e-6

kernel_name = 'hybrid_shortconv_sgu_hiermoe_block'


def rms_norm(x, g):
    xf = x.astype(jnp.float32)
    y = xf * lax.rsqrt(jnp.mean(xf * xf, axis=-1, keepdims=True) + EPS)
    return (y * g.astype(jnp.float32)).astype(x.dtype)


def layer_norm(x, g, b):
    xf = x.astype(jnp.float32)
    mu = jnp.mean(xf, axis=-1, keepdims=True)
    xc = xf - mu
    y = xc * lax.rsqrt(jnp.mean(xc * xc, axis=-1, keepdims=True) + EPS)
    return (y * g.astype(jnp.float32) + b.astype(jnp.float32)).astype(x.dtype)


def causal_depthwise_conv(z, w):
    c = z.shape[-1]
    return lax.conv_general_dilated(
        z, w[:, None, :].astype(z.dtype), window_strides=(1,),
        padding=[(CONV_K - 1, 0)], dimension_numbers=('NWC', 'WIO', 'NWC'),
        feature_group_count=c)


def short_conv_mixer(b_gate, c_gate, h_in, conv_w):
    return b_gate * causal_depthwise_conv(c_gate * h_in, conv_w)


def spatial_gating_mixer(z, ln_g, ln_b, w_s, b_s):
    bsz, s, _ = z.shape
    z = jax.nn.gelu(z)
    u, v = jnp.split(z, 2, axis=-1)
    v = layer_norm(v, ln_g, ln_b)
    chunk_id = jnp.arange(SGU_BLOCK) // CHUNK
    mask = chunk_id[None, :] <= chunk_id[:, None]
    w = jnp.where(mask[None], w_s, jnp.zeros_like(w_s)).astype(v.dtype)
    vb = v.reshape(bsz, s // SGU_BLOCK, SGU_BLOCK, SGU_HEADS, SGU_HEAD_DIM)
    vm = jnp.einsum('hij,bnjhc->bnihc', w, vb) + b_s.T.astype(v.dtype)[:, :, None]
    return u * vm.reshape(bsz, s, SGU_WIDTH)


def hierarchical_moe(x, w_rg, b_rg, w_re, b_re, w_gate, w_up, w_down):
    bsz, s, d = x.shape
    t = x.reshape(-1, d)
    tf = t.astype(jnp.float32)
    p_group = jax.nn.softmax(tf @ w_rg.astype(jnp.float32) + b_rg.astype(jnp.float32), axis=-1)
    pg, g_idx = lax.top_k(p_group, 1)
    oh_g = jax.nn.one_hot(g_idx[:, 0], N_GROUPS, dtype=jnp.float32)
    exp_logits = (tf @ w_re.astype(jnp.float32) + b_re.astype(jnp.float32)).reshape(-1, N_GROUPS, EXPERTS_PER_GROUP)
    sel_logits = jnp.einsum('tg,tge->te', oh_g, exp_logits)
    pe, e_idx = lax.top_k(jax.nn.softmax(sel_logits, axis=-1), TOP_K_INNER)
    pe = pe / jnp.sum(pe, axis=-1, keepdims=True)
    w_inner = jnp.einsum('tk,tke->te', pe, jax.nn.one_hot(e_idx, EXPERTS_PER_GROUP, dtype=jnp.float32))
    gate = ((pg * oh_g)[:, :, None] * w_inner[:, None, :]).reshape(-1, N_EXPERTS).astype(x.dtype)
    hid = jax.nn.silu(jnp.einsum('td,edf->tef', t, w_gate)) * jnp.einsum('td,edf->tef', t, w_up)
    y = jnp.einsum('tef,efd->td', hid * gate[:, :, None], w_down)
    return y.reshape(bsz, s, d)


def setup_inputs(seed: int = 0) -> dict:
    key = jax.random.key(seed)
    ks = jax.random.split(key, 20)
    f32 = jnp.float32
    nrm = lambda k, shape, scale: jax.random.normal(k, shape, f32) * scale
    L = DEPTH
    return {
        'x': jax.random.normal(ks[0], (BATCH, SEQ, D_MODEL), f32),
        'norm_mix_g': 1.0 + nrm(ks[1], (L, D_MODEL), 0.02),
        'w_in': nrm(ks[2], (L, D_MODEL, IN_COLS), D_MODEL ** -0.5),
        'conv_w': nrm(ks[3], (L, CONV_K, CONV_WIDTH), CONV_K ** -0.5),
        'sgu_ln_g': 1.0 + nrm(ks[4], (L, SGU_WIDTH), 0.02),
        'sgu_ln_b': nrm(ks[5], (L, SGU_WIDTH), 0.02),
        'sgu_w_s': nrm(ks[6], (L, SGU_HEADS, SGU_BLOCK, SGU_BLOCK), SGU_BLOCK ** -0.5),
        'sgu_b_s': 1.0 + nrm(ks[7], (L, SGU_HEADS, SGU_BLOCK), 0.02),
        'w_up_conv': nrm(ks[8], (L, CONV_WIDTH, D_MODEL), CONV_WIDTH ** -0.5),
        'w_up_sgu': nrm(ks[9], (L, SGU_WIDTH, D_MODEL), SGU_WIDTH ** -0.5),
        'w_out': nrm(ks[10], (L, D_MODEL, D_MODEL), D_MODEL ** -0.5),
        'norm_ffn_g': 1.0 + nrm(ks[11], (L, D_MODEL), 0.02),
        'w_router_group': nrm(ks[12], (L, D_MODEL, N_GROUPS), D_MODEL ** -0.5),
        'b_router_group': nrm(ks[13], (L, N_GROUPS), 0.01),
        'w_router_expert': nrm(ks[14], (L, D_MODEL, N_EXPERTS), D_MODEL ** -0.5),
        'b_router_expert': nrm(ks[15], (L, N_EXPERTS), 0.01),
        'w_exp_gate': nrm(ks[16], (L, N_EXPERTS, D_MODEL, D_EXPERT), D_MODEL ** -0.5),
        'w_exp_up': nrm(ks[17], (L, N_EXPERTS, D_MODEL, D_EXPERT), D_MODEL ** -0.5),
        'w_exp_down': nrm(ks[18], (L, N_EXPERTS, D_EXPERT, D_MODEL), D_EXPERT ** -0.5),
        'norm_final_g': 1.0 + nrm(ks[19], (D_MODEL,), 0.02),
    }


def reference(x, norm_mix_g, w_in, conv_w, sgu_ln_g, sgu_ln_b, sgu_w_s, sgu_b_s,
              w_up_conv, w_up_sgu, w_out, norm_ffn_g, w_router_group, b_router_group,
              w_router_expert, b_router_expert, w_exp_gate, w_exp_up, w_exp_down,
              norm_final_g):
    h = x
    splits = [CONV_WIDTH, 2 * CONV_WIDTH, 3 * CONV_WIDTH, 3 * CONV_WIDTH + 2 * SGU_WIDTH]
    for l in range(DEPTH):
        xn = rms_norm(h, norm_mix_g[l])
        proj = xn @ w_in[l]
        b_c, c_c, h_c, z_s, gate_logits = jnp.split(proj, splits, axis=-1)
        y_conv = short_conv_mixer(b_c, c_c, h_c, conv_w[l]) @ w_up_conv[l]
        y_sgu = spatial_gating_mixer(z_s, sgu_ln_g[l], sgu_ln_b[l], sgu_w_s[l], sgu_b_s[l]) @ w_up_sgu[l]
        g_conv, g_sgu = jnp.split(jax.nn.sigmoid(gate_logits), 2, axis=-1)
        h = h + (g_conv * y_conv + g_sgu * y_sgu) @ w_out[l]
        h = h + hierarchical_moe(rms_norm(h, norm_ffn_g[l]), w_router_group[l], b_router_group[l],
                                 w_router_expert[l], b_router_expert[l],
                                 w_exp_gate[l], w_exp_up[l], w_exp_down[l])
    return rms_norm(h, norm_final_g)
```

```python
from contextlib import ExitStack
import numpy as np
import ml_dtypes
import concourse.bass as bass
import concourse.mybir as mybir
from concourse.bass_utils import run_bass_kernel_spmd

F32 = mybir.dt.float32
BF16 = mybir.dt.bfloat16
AF = mybir.ActivationFunctionType
ALU = mybir.AluOpType
AX = mybir.AxisListType

NCORES = 8
D = 2048
SEQ = 8192
TOK = SEQ // NCORES
NT = TOK // 128
KC = D // 128
CW = 1024
SW = 1024
NE = 16
FE = 512
EPS = 1e-6
NSLOT = 3
INFLIGHT = 2

ENGS = ("pe", "act", "dve", "pool", "sp")


class Op:
    __slots__ = ("eng", "fn", "deps", "is_dma", "sem", "val", "signal", "idx", "name", "_pending")

    def __init__(self, eng, fn, name=""):
        self.eng = eng
        self.fn = fn
        self.deps = []
        self.is_dma = False
        self.sem = None
        self.val = 0
        self.signal = False
        self.idx = -1
        self.name = name


class Prog:
    def __init__(self, nc, stack):
        self.nc = nc
        self.stack = stack
        self.ops = []
        self.eng_ops = {e: [] for e in ENGS}
        self.last_writer = {}
        self.readers = {}
        self.esem = {e: stack.enter_context(nc.semaphore("prog_" + e)) for e in ENGS}
        self.dma_sems = {}
        self.dma_counts = {}
        self.regs = {}
        self.in_cond = False

    def cond_begin(self, engs, flag_idx):
        self.in_cond = True
        for e in engs:
            self.eng_ops[e].append(("begin", flag_idx))

    def cond_end(self, engs):
        self.in_cond = False
        for e in engs:
            self.eng_ops[e].append(("end",))

    def _track(self, op, reads, writes):
        qkey = lambda o: ("d", o.sem) if o.is_dma else ("e", o.eng)
        deps = []
        for k in reads:
            deps.extend(self.last_writer.get(k, {}).values())
        for k in writes:
            deps.extend(self.readers.get(k, {}).values())
            deps.extend(self.last_writer.get(k, {}).values())
        op._pending = (list(reads), list(writes))
        flat = []
        for d in deps:
            if d is op:
                continue
            if d.eng is None:
                flat.extend(d.deps)
            else:
                flat.append(d)
        best = {}
        for d in flat:
            key = qkey(d)
            cur = best.get(key)
            if cur is None or d.idx > cur.idx:
                best[key] = d
        op.deps = list(best.values())

    def _commit(self, op):
        reads, writes = op._pending
        if op.eng is None:
            k0 = ("v", op.idx)
        else:
            k0 = ("d", op.sem) if op.is_dma else ("e", op.eng)
        for k in reads:
            self.readers.setdefault(k, {})[k0] = op
        for k in writes:
            if self.in_cond and op.eng is not None:
                self.last_writer.setdefault(k, {})[k0] = op
            else:
                self.last_writer[k] = {k0: op}
                self.readers[k] = {}

    def add(self, eng, fn, reads=(), writes=(), name="", _defer=False):
        op = Op(eng, fn, name)
        op.idx = len(self.ops)
        self._track(op, reads, writes)
        if not _defer:
            self._commit(op)
        self.ops.append(op)
        self.eng_ops[eng].append(op)
        return op

    def dma(self, eng, out, in_, semname, reads=(), writes=(), name="", **kw):
        if semname not in self.dma_sems:
            self.dma_sems[semname] = self.stack.enter_context(self.nc.semaphore("d_" + semname))
            self.dma_counts[semname] = 0
        if kw.pop("indirect", False):
            fn = lambda e: e.indirect_dma_start(out=out, in_=in_, **kw)
        else:
            fn = lambda e: e.dma_start(out=out, in_=in_, **kw)
        op = self.add(eng, fn, reads, writes, name, _defer=True)
        op.is_dma = True
        op.sem = semname
        self._commit(op)
        self.dma_counts[semname] += 1
        op.val = 16 * self.dma_counts[semname]
        return op

    def fence(self, keys, eng=None):
        keys = list(keys)
        op = Op(None, None, "fence")
        op.idx = len(self.ops)
        self._track(op, keys, keys)
        self._commit(op)
        return op

    def emit(self):
        nc = self.nc
        for op in self.ops:
            for d in op.deps:
                if not d.is_dma:
                    d.signal = True
        cnt = {e: 0 for e in ENGS}
        for op in self.ops:
            if op.is_dma:
                continue
            if op.signal:
                cnt[op.eng] += 1
                op.val = cnt[op.eng]
        handles = {"pe": "tensor", "act": "scalar", "dve": "vector", "pool": "gpsimd", "sp": "sync"}
        with nc.Block() as block:
            for e in ENGS:
                ops = self.eng_ops[e]
                if not ops:
                    continue

                def body(engine, ops=ops, e=e):
                    waited = {}
                    saved = None
                    guard = None
                    n_sig = 0
                    for op in ops:
                        if isinstance(op, tuple):
                            if op[0] == "begin":
                                saved = dict(waited)
                                n_sig = 0
                                guard = engine.If_eq(self.regs[e][op[1]], 1)
                                guard.__enter__()
                            else:
                                guard.__exit__(None, None, None)
                                if n_sig:
                                    with engine.Else():
                                        engine.drain(fusable=False).then_inc(self.esem[e], n_sig)
                                waited = saved
                                guard = None
                            continue
                        for d in op.deps:
                            if d.is_dma:
                                key = ("d", d.sem)
                                sem = self.dma_sems[d.sem]
                            else:
                                key = ("e", d.eng)
                                sem = self.esem[d.eng]
                            if waited.get(key, 0) >= d.val:
                                continue
                            waited[key] = d.val
                            engine.wait_ge(sem, d.val)
                        ins = op.fn(engine) if op.fn is not None else None
                        if op.is_dma:
                            ins.then_inc(self.dma_sems[op.sem], 16)
                        elif op.signal:
                            if ins is None:
                                ins = engine.drain(fusable=False)
                            ins.then_inc(self.esem[e], 1)
                            if guard is not None:
                                n_sig += 1

                getattr(block, handles[e])(body)


def build_program(debug=(), stop_after=99):
    nc = bass.Bass("TRN2", target_bir_lowering=False)
    dram_in = lambda n, s, d=F32: nc.dram_tensor(n, list(s), d, kind="ExternalInput").ap()
    x_d = dram_in("x", [TOK, D])
    xh_d = dram_in("xh", [2, D])
    w_in_d = dram_in("w_in", [D, 9216])
    w_upc_d = dram_in("w_up_conv", [CW, D])
    w_ups_d = dram_in("w_up_sgu", [SW, D])
    w_out_d = dram_in("w_out", [D, D])
    wg_d = dram_in("w_exp_gate", [NE, D, FE])
    wu_d = dram_in("w_exp_up", [NE, D, FE])
    wd_d = dram_in("w_exp_down", [NE, FE, D])
    gmix_d = dram_in("gmix_bc", [128, D])
    gffn_d = dram_in("gffn_bc", [128, D])
    gfin_d = dram_in("gfin_bc", [128, D])
    lng_d = dram_in("lng_bc", [128, SW])
    lnb_d = dram_in("lnb_bc", [128, SW])
    cw_d = dram_in("cw", [128, 8, 3])
    wsT_d = dram_in("wsT", [128, 8, 128])
    bs_d = dram_in("bs", [8, 128])
    wr_d = dram_in("wr", [128, KC, 20])
    br_d = dram_in("br_bc", [128, 20])
    ident_d = dram_in("ident", [128, 128], BF16)
    sel_d = dram_in("sel", [8, 1024], BF16)
    tri_d = dram_in("tri", [128, 256], BF16)
    thr_d = dram_in("thr", [128, 16])
    tokid_d = dram_in("tokid", [128, NT, 16], mybir.dt.int32)
    out_d = nc.dram_tensor("out", [TOK, D], F32, kind="ExternalOutput").ap()
    hu_d = nc.dram_tensor("hu_scr", [TOK, D], F32, kind="Internal").ap()
    tnu_d = nc.dram_tensor("tnu_scr", [TOK, D], BF16, kind="Internal").ap()
    gu_d = nc.dram_tensor("gu_scr", [TOK, NE], F32, kind="Internal").ap()
    iv_d = nc.dram_tensor("iv_scr", [TOK, 16], mybir.dt.int32, kind="Internal").ap()
    dbg_out = {}

    with ExitStack() as st:
        P = Prog(nc, st)
        sb = lambda n, s, d: st.enter_context(nc.sbuf_tensor(n, list(s), d))

        R1 = sb("R1", [128, 65536], mybir.dt.uint8)
        R2 = sb("R2", [128, 36864], mybir.dt.uint8)
        xnT = sb("xnT", [128, KC, TOK], BF16)
        wring = [sb(f"wring{i}", [128, 8192], BF16) for i in range(NSLOT)]
        R3 = sb("R3", [128, 16384], mybir.dt.uint8)
        ident = sb("ident_sb", [128, 128], BF16)
        cwt = sb("cw_sb", [128, 8, 3], F32)
        bs_f = sb("bs_f", [8, 128], F32)
        bs_hi = sb("bs_hi", [8, 128], BF16)
        bs_lo = sb("bs_lo", [8, 128], BF16)
        wr_f = sb("wr_f", [128, KC, 20], F32)
        wr_hi = sb("wr_hi", [128, KC, 20], BF16)
        wr_lo = sb("wr_lo", [128, KC, 20], BF16)
        br = sb("br_sb", [128, 20], F32)
        xnTh = sb("xnTh", [128, KC, 2], BF16)
        ss = sb("ss", [128, 4, NT], F32)
        rstd = sb("rstd", [128, 4, NT], F32)
        mv = sb("mv", [128, NT, 2], F32)
        bnst = sb("bnst", [128, 2, 6], F32)
        lnr = sb("lnr", [128, NT], F32)
        rt = sb("rt", [128, 704], F32)
        gate = sb("gate", [128, NT, NE], F32)
        gate_s = sb("gate_s", [128, NT, NE], F32)
        tri = sb("tri_sb", [128, 256], BF16)
        thr = sb("thr_sb", [128, 16], F32)
        tokid = sb("tokid_sb", [128, NT, 16], mybir.dt.int32)
        inv_sb = sb("inv_sb", [128, NT, 16], mybir.dt.int32)
        dest_i = sb("dest_i", [128, NT], mybir.dt.int32)
        F_i = sb("F_i", [128, 32], mybir.dt.int32)
        ohg_bf = sb("ohg_bf", [128, NT, 4], BF16)

        def view(region, off, shape, dt):
            esz = {F32: 4, BF16: 2}[dt]
            n = int(np.prod(shape[1:]))
            ap = region[:, off:off + n * esz].bitcast(dt)
            if len(shape) == 3:
                ap = ap.rearrange("p (a b) -> p a b", b=shape[2])
            return ap

        gbc = view(R3, 0, [128, D], F32)
        wsT_f = view(R3, 8192, [128, 8, 128], F32)
        wsT = view(R3, 12288, [128, 8, 128], BF16)
        sel = view(R3, 14336, [128, 1024], BF16)[0:8, :]
        h_tm = view(R1, 0, [128, NT, D], F32)
        yA = view(R1, 0, [128, 8, TOK], BF16)
        yB = view(R1, 16384, [128, 8, TOK], BF16)
        v_tok = view(R1, 32768, [128, NT, SW], BF16)
        vg = view(R1, 49152, [128, 4, SW], F32)
        xn_bf = [view(R2, i * 4096, [128, D], BF16) for i in range(2)]
        junk = view(R2, 8192, [128, D], BF16)
        tn_f = view(R2, 12288, [128, D], F32)
        tn_f2 = view(R2, 28672, [128, D], F32)
        tn_lo = view(R2, 20480, [128, D], BF16)
        tlT = view(R2, 24576, [128, KC, 128], BF16)
        xh = view(R2, 12288, [128, D], F32)[0:2, :]
        xh_bf = view(R2, 20480, [128, D], BF16)[0:2, :]
        cz = view(R2, 0, [128, 4, TOK + 2], F32)
        acc = view(R2, 16448, [128, 4, TOK], F32)
        m_sb = view(R2, 0, [128, KC, TOK], BF16)
        hidT1 = view(R2, 0, [128, 4, TOK], BF16)
        hid_tm = view(R2, 8192, [128, NT, 512], BF16)
        sg = [view(R2, 16384 + i * 2048, [128, 512], F32) for i in range(2)]
        wring.append(R2[:, 20480:36864].bitcast(BF16))
        wring.append(R3[:, :].bitcast(BF16))
        sig_t = [view(R1, 32768 + i * 2048, [128, 512], F32) for i in range(4)]

        ps = st.enter_context(nc.psum_tensor("ps", [128, 8, 512], F32))
        bank_ptr = [0]

        def banks(n):
            b = bank_ptr[0]
            if b % n:
                b += n - b % n
            if b + n > 8:
                b = 0
            bank_ptr[0] = (b + n) % 8
            return b

        PS = lambda b: ("ps", b)

        def dump(name, ap, shape, dt, reads):
            if name not in debug:
                return
            t = nc.dram_tensor("dbg_" + name, list(shape), dt, kind="ExternalOutput").ap()
            dbg_out[name] = t
            P.dma("sp", t, ap, "dbg_" + name, reads=reads, writes=[("dbg", name)])

        w_in_v = w_in_d.rearrange("(kc p) c -> p kc c", p=128)
        upc_v = w_upc_d.rearrange("(kc p) c -> p kc c", p=128)
        ups_v = w_ups_d.rearrange("(kc p) c -> p kc c", p=128)
        wout_v = w_out_d.rearrange("(kc p) c -> p kc c", p=128)
        pieces = []

        def piece_in(c0):
            pieces.append((w_in_v[:, :, c0:c0 + 512], KC, 512))

        for half in range(2):
            piece_in(1024 + half * 512)
            piece_in(2048 + half * 512)
            piece_in(0 + half * 512)
        piece_in(3072); piece_in(3584)
        piece_in(4096); piece_in(4608)
        for dg in range(4):
            piece_in(5120 + dg * 512)
            pieces.append((upc_v[:, :, dg * 512:(dg + 1) * 512], 8, 512))
            piece_in(7168 + dg * 512)
            pieces.append((ups_v[:, :, dg * 512:(dg + 1) * 512], 8, 512))
        for n in range(4):
            pieces.append((wout_v[:, :, n * 512:(n + 1) * 512], KC, 512))
        def gu_pieces(e):
            pieces.append((wg_d[e].rearrange("(kc p) f -> p kc f", p=128), KC, 512))
            pieces.append((wu_d[e].rearrange("(kc p) f -> p kc f", p=128), KC, 512))
        gu_pieces(0)
        for e in range(NE):
            if e + 1 < NE:
                gu_pieces(e + 1)
            pieces.append((wd_d[e].rearrange("(kc p) f -> p kc f", p=128), 4, 2048))
        issued = [0]
        cur = [0]
        released = [0]
        n_mixer = len(pieces) - 3 * NE
        piece_slot = [i % 3 if i < n_mixer else (i - n_mixer) % 5 for i in range(len(pieces))]
        slot_owner = {0: None, 1: None, 2: None, 3: -1, 4: -1}

        def slot_view(i):
            _, kc, cols = pieces[i]
            return wring[piece_slot[i]][:, 0:kc * cols].rearrange("p (kc c) -> p kc c", c=cols)

        def try_issue():
            while issued[0] < len(pieces):
                i = issued[0]
                sl = piece_slot[i]
                if slot_owner[sl] is not None:
                    break
                slot_owner[sl] = i
                issued[0] += 1
                P.dma("pool", slot_view(i), pieces[i][0], f"w{sl}", writes=[("w", sl), ("wtok", i % INFLIGHT)], name=f"wpiece{i}")

        def w_acquire():
            i = cur[0]
            cur[0] += 1
            assert i < issued[0], "weight piece not yet issued (ring too small for this access pattern)"
            return slot_view(i), ("w", piece_slot[i])

        def w_release():
            i = released[0]
            released[0] += 1
            slot_owner[piece_slot[i]] = None
            try_issue()

        def enable_slot3():
            slot_owner[3] = None
            slot_owner[4] = None
            try_issue()

        issue_next = try_issue

        P.dma("sp", ident[:], ident_d, "c_ident", writes=["ident"])
        P.dma("act", gbc[:], gmix_d, "c_gbc", writes=["gbc"])
        for t in range(NT):
            P.dma("sp" if t % 2 == 0 else "act", h_tm[:, t, :], x_d[t * 128:(t + 1) * 128, :], f"x{t}", writes=[("h", t)])
        P.dma("sp", xh[:], xh_d, "c_xh", writes=["xh"])
        P.dma("act", cwt[:], cw_d, "c_cw", writes=["cw"])
        P.dma("sp", wsT_f[:], wsT_d, "c_ws", writes=["wsT_f"])
        P.dma("act", bs_f[:], bs_d, "c_bs", writes=["bs_f"])
        P.dma("sp", sel[:], sel_d, "c_sel", writes=["sel"])
        P.dma("act", wr_f[:], wr_d, "c_wr", writes=["wr_f"])
        P.dma("sp", br[:], br_d, "c_br", writes=["br"])
        P.dma("act", tri[:], tri_d, "c_tri", writes=["tri"])
        P.dma("sp", thr[:], thr_d, "c_thr", writes=["thr"])
        P.dma("act", tokid[:], tokid_d, "c_tokid", writes=["tokid"])
        try_issue()

        P.add("dve", lambda e: e.memset(ss[:], 0.0), writes=["ss"])
        def rms_stats(src_tile, nrm, t):
            if True:
                P.add("act", lambda e, t=t: e.activation(out=junk[:], in_=src_tile(t), func=AF.Square,
                                                         accum_out=ss[:, nrm, t:t + 1]),
                      reads=[("h", t), "ss"], writes=["junk", ("ss", nrm, t)])
                P.add("act", lambda e, t=t: e.activation(out=rstd[:, nrm, t:t + 1], in_=ss[:, nrm, t:t + 1], func=AF.Sqrt,
                                                         scale=1.0 / D, bias=eps_t[:, 0:1]),
                      reads=[("ss", nrm, t), "eps"], writes=[("rstd", nrm, t)])
                P.add("dve", lambda e, t=t: e.reciprocal(out=rstd[:, nrm, t:t + 1], in_=rstd[:, nrm, t:t + 1]),
                      reads=[("rstd", nrm, t)], writes=[("rstd", nrm, t)])

        eps_t = sb("eps_t", [128, 1], F32)
        P.add("dve", lambda e: e.memset(eps_t[:], EPS), writes=["eps"])

        def transpose_tile(src_bf, dst, t, src_key, dst_key, rows=128, width=128, evac_alt=0):
            for hb in range(2):
                b = banks(1)
                pst = ps[:, b, :].bitcast(BF16)

                def trg(e, hb=hb, pst=pst):
                    ins = None
                    for j in range(8):
                        k = hb * 8 + j
                        ins = e.transpose(out=pst[:, j * width:(j + 1) * width],
                                          in_=src_bf[0:rows, k * 128:(k + 1) * 128],
                                          identity=ident[0:rows, 0:rows])
                    return ins
                P.add("pe", trg, reads=[src_key, "ident"], writes=[PS(b)])
                src_v = pst[:, 0:8 * width].rearrange("p (j c) -> p j c", c=width)
                dst_v = dst[:, hb * 8:(hb + 1) * 8, t * width:(t + 1) * width]
                if (hb + evac_alt) % 2 == 0:
                    P.add("act", lambda e, s=src_v, d=dst_v: e.copy(out=d, in_=s), reads=[PS(b)], writes=[dst_key])
                else:
                    P.add("dve", lambda e, s=src_v, d=dst_v: e.tensor_copy(out=d, in_=s), reads=[PS(b)], writes=[dst_key])

        def mm_A(wv, wkey, nk, col0, rhs_fn, rhs_keys, halo=None):
            b0 = banks(2)
            bh = banks(1) if halo is not None else None

            def g(e):
                ins = None
                for k in range(nk):
                    lhs = wv[:, k, col0:col0 + 128]
                    for n2 in range(2):
                        ins = e.matmul(ps[:, b0 + n2, :], lhsT=lhs, rhs=rhs_fn(k, n2), start=(k == 0), stop=(k == nk - 1))
                    if halo is not None:
                        ins = e.matmul(ps[:, bh, 0:2], lhsT=lhs, rhs=halo(k), start=(k == 0), stop=(k == nk - 1))
                return ins
            wr = [PS(b0), PS(b0 + 1)] + ([PS(bh)] if halo is not None else [])
            P.add("pe", g, reads=[wkey] + list(rhs_keys), writes=wr)
            return b0, bh

        xn_rhs = lambda k, n2: xnT[:, k, n2 * 512:(n2 + 1) * 512]
        xnT_keys = [("xnT", t) for t in range(NT)]

        rms_stats(lambda t: h_tm[:, t, :], 0, 0)
        for t in range(NT):
            if t + 1 < NT:
                rms_stats(lambda t: h_tm[:, t, :], 0, t + 1)
            xb = xn_bf[t % 2]
            P.add("dve", lambda e, t=t, xb=xb: e.scalar_tensor_tensor(out=xb, in0=h_tm[:, t, :], scalar=rstd[:, 0, t:t + 1],
                                                                      in1=gbc[:], op0=ALU.mult, op1=ALU.mult),
                  reads=[("h", t), ("rstd", 0, t), "gbc"], writes=[("xn_bf", t % 2)])
            transpose_tile(xb, xnT, t, ("xn_bf", t % 2), ("xnT", t), evac_alt=t)
        P.add("act", lambda e: e.activation(out=junk[0:2, :], in_=xh[:], func=AF.Square, accum_out=ss[0:2, 3, 0:1]),
              reads=["xh", "ss"], writes=["junk", "ssh"])
        P.add("act", lambda e: e.activation(out=rstd[0:2, 3, 0:1], in_=ss[0:2, 3, 0:1], func=AF.Sqrt, scale=1.0 / D, bias=eps_t[0:2, 0:1]),
              reads=["ssh", "eps"], writes=["rstdh"])
        P.add("dve", lambda e: e.reciprocal(out=rstd[0:2, 3, 0:1], in_=rstd[0:2, 3, 0:1]), reads=["rstdh"], writes=["rstdh"])
        P.add("dve", lambda e: e.scalar_tensor_tensor(out=xh_bf[:], in0=xh[:], scalar=rstd[0:2, 3, 0:1], in1=gbc[0:2, :],
                                                      op0=ALU.mult, op1=ALU.mult),
              reads=["xh", "rstdh", "gbc"], writes=["xh_bf"])
        transpose_tile(xh_bf, xnTh, 0, "xh_bf", "xnTh", rows=2, width=2)
        P.add("dve", lambda e: e.tensor_copy(out=wsT[:], in_=wsT_f[:]), reads=["wsT_f"], writes=["wsT"])
        P.add("dve", lambda e: e.memset(wsT[64:128, :, 0:64], 0.0), writes=["wsT"])
        P.add("dve", lambda e: e.tensor_copy(out=bs_hi[:], in_=bs_f[:]), reads=["bs_f"], writes=["bs_hi"])
        P.add("dve", lambda e: e.tensor_tensor(out=bs_lo[:], in0=bs_f[:], in1=bs_hi[:], op=ALU.subtract),
              reads=["bs_f", "bs_hi"], writes=["bs_lo"])
        P.add("dve", lambda e: e.tensor_copy(out=wr_hi[:], in_=wr_f[:]), reads=["wr_f"], writes=["wr_hi"])
        P.add("dve", lambda e: e.tensor_tensor(out=wr_lo[:], in0=wr_f[:], in1=wr_hi[:], op=ALU.subtract),
              reads=["wr_f", "wr_hi"], writes=["wr_lo"])

        dump("xnT", xnT[:], [128, KC, TOK], BF16, xnT_keys)
        dump("xnTh", xnTh[:], [128, KC, 2], BF16, ["xnTh"])

        R1_keys = [("h", t) for t in range(NT)]
        P.fence(R1_keys + ["yA"] + [("yB", j) for j in range(8)] + [("v_tok", t) for t in range(NT)] + [("vg", q) for q in range(4)])
        P.fence(["junk", ("xn_bf", 0), ("xn_bf", 1), "xh", "xh_bf"] + [("cz", j) for j in range(4)] + [("acc", j) for j in range(4)])
        P.dma("sp", gbc[:, 0:SW], lng_d, "c_gbc", reads=[], writes=["gbc"])
        P.dma("sp", gbc[:, SW:2 * SW], lnb_d, "c_gbc", reads=[], writes=["gbc"])

        if stop_after >= 1:
            halo_rhs = lambda k: xnTh[:, k, :]
            for half in range(2):
                wv, wk = w_acquire()
                for jj in range(4):
                    b0, bh = mm_A(wv, wk, KC, jj * 128, xn_rhs, xnT_keys + ["xnTh"], halo=halo_rhs)
                    for n2 in range(2):
                        P.add("act", lambda e, jj=jj, n2=n2, b0=b0: e.copy(out=cz[:, jj, 2 + n2 * 512:2 + (n2 + 1) * 512], in_=ps[:, b0 + n2, :]),
                              reads=[PS(b0 + n2)], writes=[("cz", jj)])
                    P.add("act", lambda e, jj=jj, bh=bh: e.copy(out=cz[:, jj, 0:2], in_=ps[:, bh, 0:2]), reads=[PS(bh)], writes=[("cz", jj)])
                w_release()
                wv, wk = w_acquire()
                for jj in range(4):
                    j = half * 4 + jj
                    b0, bh = mm_A(wv, wk, KC, jj * 128, xn_rhs, xnT_keys + ["xnTh"], halo=halo_rhs)
                    for n2 in range(2):
                        P.add("dve", lambda e, jj=jj, n2=n2, b0=b0: e.tensor_tensor(
                            out=cz[:, jj, 2 + n2 * 512:2 + (n2 + 1) * 512], in0=ps[:, b0 + n2, :],
                            in1=cz[:, jj, 2 + n2 * 512:2 + (n2 + 1) * 512], op=ALU.mult),
                            reads=[PS(b0 + n2), ("cz", jj)], writes=[("cz", jj)])
                    P.add("dve", lambda e, jj=jj, bh=bh: e.tensor_tensor(out=cz[:, jj, 0:2], in0=ps[:, bh, 0:2], in1=cz[:, jj, 0:2], op=ALU.mult),
                          reads=[PS(bh), ("cz", jj)], writes=[("cz", jj)])
                    P.add("act", lambda e, jj=jj, j=j: e.mul(out=acc[:, jj, :], in_=cz[:, jj, 2:TOK + 2], mul=cwt[:, j, 2:3]),
                          reads=[("cz", jj), "cw"], writes=[("acc", jj)])
                    P.add("dve", lambda e, jj=jj, j=j: e.scalar_tensor_tensor(out=acc[:, jj, :], in0=cz[:, jj, 1:TOK + 1], scalar=cwt[:, j, 1:2],
                                                                              in1=acc[:, jj, :], op0=ALU.mult, op1=ALU.add),
                          reads=[("cz", jj), "cw", ("acc", jj)], writes=[("acc", jj)])
                    P.add("dve", lambda e, jj=jj, j=j: e.scalar_tensor_tensor(out=acc[:, jj, :], in0=cz[:, jj, 0:TOK], scalar=cwt[:, j, 0:1],
                                                                              in1=acc[:, jj, :], op0=ALU.mult, op1=ALU.add),
                          reads=[("cz", jj), "cw", ("acc", jj)], writes=[("acc", jj)])
                w_release()
                wv, wk = w_acquire()
                for jj in range(4):
                    j = half * 4 + jj
                    b0, _ = mm_A(wv, wk, KC, jj * 128, xn_rhs, xnT_keys)
                    for n2 in range(2):
                        P.add("dve", lambda e, jj=jj, j=j, n2=n2, b0=b0: e.tensor_tensor(
                            out=yA[:, j, n2 * 512:(n2 + 1) * 512], in0=ps[:, b0 + n2, :],
                            in1=acc[:, jj, n2 * 512:(n2 + 1) * 512], op=ALU.mult),
                            reads=[PS(b0 + n2), ("acc", jj)], writes=["yA"])
                w_release()
            dump("yA", yA, [128, 8, TOK], BF16, ["yA"])

        if stop_after >= 2:
            for half in range(2):
                wv, wk = w_acquire()
                for jj in range(4):
                    j = half * 4 + jj
                    b0, _ = mm_A(wv, wk, KC, jj * 128, xn_rhs, xnT_keys)
                    for n2 in range(2):
                        P.add("act", lambda e, j=j, n2=n2, b0=b0: e.activation(out=yB[:, j, n2 * 512:(n2 + 1) * 512], in_=ps[:, b0 + n2, :],
                                                                              func=AF.Gelu_apprx_tanh),
                              reads=[PS(b0 + n2)], writes=[("yB", j)])
                w_release()
            wv0, wk0 = w_acquire()
            wv1, wk1 = w_acquire()
            for hq in range(2):
                for tq in range(4):
                    t = hq * 4 + tq
                    bv = banks(2)

                    def g(e, t=t, bv=bv):
                        ins = None
                        for k in range(KC):
                            lhs = xnT[:, k, t * 128:(t + 1) * 128]
                            e.matmul(ps[:, bv, :], lhsT=lhs, rhs=wv0[:, k, :], start=(k == 0), stop=(k == KC - 1))
                            ins = e.matmul(ps[:, bv + 1, :], lhsT=lhs, rhs=wv1[:, k, :], start=(k == 0), stop=(k == KC - 1))
                        return ins
                    P.add("pe", g, reads=[wk0, wk1, ("xnT", t)], writes=[PS(bv), PS(bv + 1)])
                    for pv in range(2):
                        b = bv + pv
                        P.add("act", lambda e, tq=tq, pv=pv, b=b: e.activation(out=vg[:, tq, pv * 512:(pv + 1) * 512], in_=ps[:, b, :],
                                                                              func=AF.Gelu_apprx_tanh),
                              reads=[PS(b)], writes=[("vg", tq)])
                        P.add("dve", lambda e, tq=tq, pv=pv: e.bn_stats(out=bnst[:, pv, :], in_=vg[:, tq, pv * 512:(pv + 1) * 512]),
                              reads=[("vg", tq)], writes=[("bnst", pv)])
                    P.add("dve", lambda e, t=t: e.bn_aggr(out=mv[:, t, :], in_=bnst[:].rearrange("p a b -> p (a b)")),
                          reads=[("bnst", 0), ("bnst", 1)], writes=[("mv", t)])
                sl = slice(hq * 4, hq * 4 + 4)
                P.add("act", lambda e, sl=sl: e.activation(out=lnr[:, sl], in_=mv[:, sl, 1], func=AF.Sqrt, bias=eps_t[:, 0:1]),
                      reads=[("mv", t) for t in range(hq * 4, hq * 4 + 4)] + ["eps"], writes=[("lnr", hq)])
                P.add("dve", lambda e, sl=sl: e.reciprocal(out=lnr[:, sl], in_=lnr[:, sl]), reads=[("lnr", hq)], writes=[("lnr", hq)])
                for tq in range(4):
                    t = hq * 4 + tq
                    P.add("dve", lambda e, t=t, tq=tq: e.tensor_scalar(out=vg[:, tq, :], in0=vg[:, tq, :], scalar1=mv[:, t, 0:1],
                                                                      scalar2=lnr[:, t:t + 1], op0=ALU.subtract, op1=ALU.mult),
                          reads=[("vg", tq), ("mv", t), ("lnr", hq)], writes=[("vg", tq)])
                    P.add("dve", lambda e, tq=tq: e.tensor_tensor(out=vg[:, tq, :], in0=vg[:, tq, :], in1=gbc[:, 0:SW], op=ALU.mult),
                          reads=[("vg", tq), "gbc"], writes=[("vg", tq)])
                    P.add("dve", lambda e, t=t, tq=tq: e.tensor_tensor(out=v_tok[:, t, :], in0=vg[:, tq, :], in1=gbc[:, SW:2 * SW], op=ALU.add),
                          reads=[("vg", tq), "gbc"], writes=[("v_tok", t)])
                    for hg in range(2):
                        b = banks(1)

                        def g(e, t=t, hg=hg, b=b):
                            ins = None
                            for hh in range(4):
                                hd = hg * 4 + hh
                                o = ps[:, b, hh * 128:(hh + 1) * 128]
                                e.matmul(o, lhsT=v_tok[:, t, hd * 128:(hd + 1) * 128], rhs=wsT[:, hd, :], start=True, stop=False)
                                e.matmul(o, lhsT=sel[:, hd * 128:(hd + 1) * 128], rhs=bs_hi[:], start=False, stop=False)
                                ins = e.matmul(o, lhsT=sel[:, hd * 128:(hd + 1) * 128], rhs=bs_lo[:], start=False, stop=True)
                            return ins
                        P.add("pe", g, reads=[("v_tok", t), "wsT", "sel", "bs_hi", "bs_lo"], writes=[PS(b)])
                        P.add("dve", lambda e, t=t, hg=hg, b=b: e.tensor_tensor(
                            out=yB[:, hg * 4:(hg + 1) * 4, t * 128:(t + 1) * 128],
                            in0=ps[:, b, :].rearrange("p (h c) -> p h c", c=128),
                            in1=yB[:, hg * 4:(hg + 1) * 4, t * 128:(t + 1) * 128], op=ALU.mult),
                            reads=[PS(b)] + [("yB", hg * 4 + i) for i in range(4)], writes=[("yB", hg * 4 + i) for i in range(4)])
            w_release()
            w_release()
            dump("v_tok", v_tok, [128, NT, SW], BF16, [("v_tok", t) for t in range(NT)])
            dump("yB", yB, [128, 8, TOK], BF16, [("yB", j) for j in range(8)])

        if stop_after >= 3:
            P.fence(["cz", "acc"] + [("cz", j) for j in range(4)] + [("acc", j) for j in range(4)] + [("m", d) for d in range(KC)])
            P.fence([("v_tok", t) for t in range(NT)] + [("vg", q) for q in range(4)]
                    + [("sig", i) for i in range(4)] + [("tc", jj, n2) for jj in range(4) for n2 in range(2)])
            tc_t = {(jj, n2): view(R1, 40960 + (jj * 2 + n2) * 2048, [128, 512], F32) for jj in range(4) for n2 in range(2)}
            yA_rhs = lambda k, n2: yA[:, k, n2 * 512:(n2 + 1) * 512]
            yB_rhs = lambda k, n2: yB[:, k, n2 * 512:(n2 + 1) * 512]
            yB_keys = [("yB", j) for j in range(8)]
            for dg in range(4):
                for br_i in range(2):
                    wg_, kg_ = w_acquire()
                    wu_, ku_ = w_acquire()
                    for jj in range(4):
                        d = dg * 4 + jj
                        bg, _ = mm_A(wg_, kg_, KC, jj * 128, xn_rhs, xnT_keys)
                        for n2 in range(2):
                            si = (jj % 2) * 2 + n2
                            P.add("act", lambda e, bg=bg, n2=n2, si=si: e.activation(out=sig_t[si], in_=ps[:, bg + n2, :], func=AF.Sigmoid),
                                  reads=[PS(bg + n2)], writes=[("sig", si)])
                        if br_i == 0:
                            by, _ = mm_A(wu_, ku_, 8, jj * 128, yA_rhs, ["yA"])
                        else:
                            by, _ = mm_A(wu_, ku_, 8, jj * 128, yB_rhs, yB_keys)
                        for n2 in range(2):
                            si = (jj % 2) * 2 + n2
                            tt = tc_t[(jj, n2)]
                            if br_i == 0:
                                P.add("dve", lambda e, by=by, n2=n2, si=si, tt=tt: e.tensor_tensor(out=tt, in0=ps[:, by + n2, :], in1=sig_t[si], op=ALU.mult),
                                      reads=[PS(by + n2), ("sig", si)], writes=[("tc", jj, n2)])
                            else:
                                mo = m_sb[:, d, n2 * 512:(n2 + 1) * 512]
                                P.add("dve", lambda e, by=by, n2=n2, si=si: e.tensor_tensor(out=sig_t[si], in0=ps[:, by + n2, :], in1=sig_t[si], op=ALU.mult),
                                      reads=[PS(by + n2), ("sig", si)], writes=[("sig", si)])
                                P.add("dve", lambda e, si=si, tt=tt, mo=mo: e.tensor_tensor(out=mo, in0=sig_t[si], in1=tt, op=ALU.add),
                                      reads=[("sig", si), ("tc", jj, n2)], writes=[("m", d)])
                    w_release()
                    w_release()
            dump("m", m_sb, [128, KC, TOK], BF16, [("m", d) for d in range(KC)])

        if stop_after >= 4:
            P.fence(["yA"] + [("yB", j) for j in range(8)] + [("sig", i) for i in range(4)]
                    + [("tc", jj, n2) for jj in range(4) for n2 in range(2)] + R1_keys)
            for t in range(NT):
                P.dma("sp" if t % 2 == 0 else "act", h_tm[:, t, :], x_d[t * 128:(t + 1) * 128, :], f"x{t}", writes=[("h", t)])
            m_keys = [("m", d) for d in range(KC)]
            for n in range(4):
                wv, wk = w_acquire()
                for t in range(NT):
                    b = banks(1)

                    def g(e, t=t, wv=wv, b=b):
                        ins = None
                        for k in range(KC):
                            ins = e.matmul(ps[:, b, :], lhsT=m_sb[:, k, t * 128:(t + 1) * 128], rhs=wv[:, k, :], start=(k == 0), stop=(k == KC - 1))
                        return ins
                    P.add("pe", g, reads=[wk] + m_keys, writes=[PS(b)])
                    P.add("dve", lambda e, t=t, n=n, b=b: e.tensor_tensor(out=h_tm[:, t, n * 512:(n + 1) * 512], in0=ps[:, b, :],
                                                                         in1=h_tm[:, t, n * 512:(n + 1) * 512], op=ALU.add),
                          reads=[PS(b), ("h", t)], writes=[("h", t)])
                    if n == 3:
                        P.dma("sp" if t % 2 == 0 else "act", hu_d[t * 128:(t + 1) * 128, :], h_tm[:, t, :], f"hu{t}",
                              reads=[("h", t)], writes=[("hu", t)])
                w_release()
            dump("h1", h_tm, [128, NT, D], F32, R1_keys)

        if stop_after >= 5:
            P.fence(m_keys + ["junk", ("xn_bf", 0), ("xn_bf", 1), ("tn_f", 0), ("tn_f", 1), "tn_lo", "tlT"])
            P.dma("sp", gbc[:], gffn_d, "c_gbc", writes=["gbc"])
            Lg = rt[:, 0:160].rearrange("p (t c) -> p t c", c=20)
            rms_stats(lambda t: h_tm[:, t, :], 1, 0)
            for t in range(NT):
                if t + 1 < NT:
                    rms_stats(lambda t: h_tm[:, t, :], 1, t + 1)
                xb = xn_bf[t % 2]
                tnf = (tn_f, tn_f2)[t % 2]
                tnk = ("tn_f", t % 2)
                P.add("dve", lambda e, t=t, tnf=tnf: e.scalar_tensor_tensor(out=tnf, in0=h_tm[:, t, :], scalar=rstd[:, 1, t:t + 1], in1=gbc[:],
                                                                   op0=ALU.mult, op1=ALU.mult),
                      reads=[("h", t), ("rstd", 1, t), "gbc"], writes=[tnk])
                P.add("act", lambda e, xb=xb, tnf=tnf: e.copy(out=xb, in_=tnf), reads=[tnk], writes=[("xn_bf", t % 2)])
                P.dma("sp", tnu_d[t * 128:(t + 1) * 128, :], xb, f"tnu{t % 2}", reads=[("xn_bf", t % 2)], writes=[("tnu", t)])
                P.add("dve", lambda e, xb=xb, tnf=tnf: e.tensor_tensor(out=tn_lo, in0=tnf, in1=xb, op=ALU.subtract),
                      reads=[tnk, ("xn_bf", t % 2)], writes=["tn_lo"])
                transpose_tile(xb, xnT, t, ("xn_bf", t % 2), ("xnT", t), evac_alt=t)
                transpose_tile(tn_lo, tlT, 0, "tn_lo", "tlT", evac_alt=t)
                b = banks(1)

                def g(e, t=t, b=b):
                    ins = None
                    o = ps[:, b, 0:20]
                    n_mm = 3 * KC
                    i = 0
                    for (lt, wt) in ((0, wr_hi), (1, wr_hi), (0, wr_lo)):
                        for k in range(KC):
                            lhs = xnT[:, k, t * 128:(t + 1) * 128] if lt == 0 else tlT[:, k, :]
                            ins = e.matmul(o, lhsT=lhs, rhs=wt[:, k, :], start=(i == 0), stop=(i == n_mm - 1))
                            i += 1
                    return ins
                P.add("pe", g, reads=[("xnT", t), "tlT", "wr_hi", "wr_lo"], writes=[PS(b)])
                P.add("dve", lambda e, t=t, b=b: e.tensor_tensor(out=Lg[:, t, :], in0=ps[:, b, 0:20], in1=br[:], op=ALU.add),
                      reads=[PS(b), "br"], writes=["Lg"])
            dump("tT", xnT[:], [128, KC, TOK], BF16, xnT_keys)
            dump("Lg", rt[:, 0:160], [128, 160], F32, ["Lg"])

            o = [160]

            def tmp(n, c=None):
                a = rt[:, o[0]:o[0] + n]
                o[0] += n
                return a if c is None else a.rearrange("p (t c) -> p t c", c=c)
            lg = Lg[:, :, 0:4]
            le = Lg[:, :, 4:20]
            mg = tmp(8); ohg = tmp(32, 4); eg = tmp(32, 4); sumg = tmp(8); pg = tmp(8)
            selv = tmp(32, 4); tm4 = tmp(32, 4); m1 = tmp(8); mk1 = tmp(32, 4); sel2 = tmp(32, 4)
            m2 = tmp(8); mk2 = tmp(32, 4); dd = tmp(8); e2 = tmp(8); w1 = tmp(8); w2 = tmp(8)
            winn = tmp(32, 4); ogp = tmp(32, 4)
            bc = lambda a: a.unsqueeze(2).to_broadcast([128, NT, 4])
            R = ["Lg", "rt"]
            V = lambda fn: P.add("dve", fn, reads=R, writes=["rt"])
            V(lambda e: e.tensor_reduce(out=mg, in_=lg, axis=AX.X, op=ALU.max))
            V(lambda e: e.tensor_tensor(out=ohg, in0=lg, in1=bc(mg), op=ALU.is_equal))

        if stop_after >= 6:
            IOA = bass.IndirectOffsetOnAxis
            V(lambda e: e.tensor_copy(out=ohg_bf[:], in_=ohg))
            b = banks(1)
            ones_bf = tri[:, 0:128]
            U_bf = tri[:, 128:256]

            def g(e, b=b):
                ins = None
                for t in range(NT):
                    ins = e.matmul(ps[:, b, 0:4], lhsT=ones_bf, rhs=ohg_bf[:, t, :], start=(t == 0), stop=(t == NT - 1))
                for t in range(NT):
                    o = ps[:, b, 4 + 4 * t:8 + 4 * t]
                    for t2 in range(t):
                        ins = e.matmul(o, lhsT=ones_bf, rhs=ohg_bf[:, t2, :], start=(t2 == 0), stop=False)
                    ins = e.matmul(o, lhsT=U_bf, rhs=ohg_bf[:, t, :], start=(t == 0), stop=True)
                return ins
            P.add("pe", g, reads=["rt", "tri"], writes=[PS(b)])
            cnt = tmp(4); start = tmp(4); end = tmp(4); rk = tmp(32, 4); dest_f = tmp(8); Ff = tmp(32, 8); tm8 = tmp(8)
            Rb = ["rt", "Lg", PS(b), "thr"]
            Vb = lambda fn: P.add("dve", fn, reads=Rb, writes=["rt"])
            Vb(lambda e: e.tensor_copy(out=cnt, in_=ps[:, b, 0:4]))
            Vb(lambda e: e.memset(start[:, 0:1], 0.0))
            Vb(lambda e: e.tensor_copy(out=start[:, 1:2], in_=cnt[:, 0:1]))
            Vb(lambda e: e.tensor_tensor(out=start[:, 2:3], in0=start[:, 1:2], in1=cnt[:, 1:2], op=ALU.add))
            Vb(lambda e: e.tensor_tensor(out=start[:, 3:4], in0=start[:, 2:3], in1=cnt[:, 2:3], op=ALU.add))
            Vb(lambda e: e.tensor_tensor(out=end, in0=start, in1=cnt, op=ALU.add))
            Vb(lambda e: e.tensor_tensor(out=rk, in0=ps[:, b, 4:36].rearrange("p (t c) -> p t c", c=4),
                                         in1=start.unsqueeze(1).to_broadcast([128, NT, 4]), op=ALU.add))
            Vb(lambda e: e.tensor_tensor(out=rk, in0=rk, in1=ohg, op=ALU.mult))
            Vb(lambda e: e.tensor_reduce(out=dest_f, in_=rk, axis=AX.X, op=ALU.add))
            P.add("dve", lambda e: e.tensor_copy(out=dest_i[:], in_=dest_f), reads=["rt"], writes=["dest_i"])
            for gi in range(4):
                Vb(lambda e, gi=gi: e.tensor_scalar(out=Ff[:, gi, :], in0=thr[:, 8:16], scalar1=start[:, gi:gi + 1], scalar2=None, op0=ALU.is_gt))
                Vb(lambda e, gi=gi: e.tensor_scalar(out=tm8, in0=thr[:, 0:8], scalar1=end[:, gi:gi + 1], scalar2=None, op0=ALU.is_lt))
                Vb(lambda e, gi=gi: e.tensor_tensor(out=Ff[:, gi, :], in0=Ff[:, gi, :], in1=tm8, op=ALU.mult))
            P.add("dve", lambda e: e.tensor_copy(out=F_i[:], in_=Ff.rearrange("p g j -> p (g j)")), reads=["rt"], writes=["flags"])
            dump("dest", dest_i[:], [128, NT], mybir.dt.int32, ["dest_i"])
            dump("flags", F_i[:], [128, 32], mybir.dt.int32, ["flags"])
            for t in range(NT):
                P.dma("pool", iv_d, tokid[:, t, :], "iv", indirect=True, out_offset=IOA(ap=dest_i[:, t:t + 1], axis=0), in_offset=None,
                      reads=["dest_i", "tokid"], writes=[("iv_d", t)])
            P.dma("sp", inv_sb[:], iv_d.rearrange("(j p) c -> p j c", p=128), "ivl", reads=[("iv_d", t) for t in range(NT)], writes=["inv"])
            dump("inv", inv_sb[:], [128, NT, 16], mybir.dt.int32, ["inv"])
            V(lambda e: e.tensor_tensor(out=eg, in0=lg, in1=bc(mg), op=ALU.subtract))
            P.add("act", lambda e: e.activation(out=eg, in_=eg, func=AF.Exp), reads=R, writes=["rt"])
            V(lambda e: e.tensor_reduce(out=sumg, in_=eg, axis=AX.X, op=ALU.add))
            V(lambda e: e.reciprocal(out=pg, in_=sumg))
            for gi in range(4):
                if gi == 0:
                    V(lambda e: e.tensor_tensor(out=selv, in0=le[:, :, 0:4], in1=bc(ohg[:, :, 0]), op=ALU.mult))
                else:
                    V(lambda e, gi=gi: e.tensor_tensor(out=tm4, in0=le[:, :, 4 * gi:4 * gi + 4], in1=bc(ohg[:, :, gi]), op=ALU.mult))
                    V(lambda e: e.tensor_tensor(out=selv, in0=selv, in1=tm4, op=ALU.add))
            V(lambda e: e.tensor_reduce(out=m1, in_=selv, axis=AX.X, op=ALU.max))
            V(lambda e: e.tensor_tensor(out=mk1, in0=selv, in1=bc(m1), op=ALU.is_equal))
            V(lambda e: e.scalar_tensor_tensor(out=sel2, in0=mk1, scalar=-1e30, in1=selv, op0=ALU.mult, op1=ALU.add))
            V(lambda e: e.tensor_reduce(out=m2, in_=sel2, axis=AX.X, op=ALU.max))
            V(lambda e: e.tensor_tensor(out=mk2, in0=sel2, in1=bc(m2), op=ALU.is_equal))
            V(lambda e: e.tensor_tensor(out=dd, in0=m2, in1=m1, op=ALU.subtract))
            P.add("act", lambda e: e.activation(out=e2, in_=dd, func=AF.Exp), reads=R, writes=["rt"])
            V(lambda e: e.tensor_scalar(out=w1, in0=e2, scalar1=1.0, scalar2=None, op0=ALU.add))
            V(lambda e: e.reciprocal(out=w1, in_=w1))
            V(lambda e: e.tensor_tensor(out=w2, in0=e2, in1=w1, op=ALU.mult))
            V(lambda e: e.tensor_tensor(out=winn, in0=mk1, in1=bc(w1), op=ALU.mult))
            V(lambda e: e.tensor_tensor(out=tm4, in0=mk2, in1=bc(w2), op=ALU.mult))
            V(lambda e: e.tensor_tensor(out=winn, in0=winn, in1=tm4, op=ALU.add))
            V(lambda e: e.tensor_tensor(out=ogp, in0=ohg, in1=bc(pg), op=ALU.mult))
            for gi in range(4):
                P.add("dve", lambda e, gi=gi: e.tensor_tensor(out=gate[:, :, 4 * gi:4 * gi + 4], in0=winn, in1=bc(ogp[:, :, gi]), op=ALU.mult),
                      reads=R, writes=["gate"])
            dump("gate", gate[:], [128, NT, NE], F32, ["gate"])
            P.dma("sp", gu_d.rearrange("(t p) c -> p t c", p=128), gate[:], "gu", reads=["gate"], writes=["gu"])
            P.fence(["junk", ("tn_f", 0), ("tn_f", 1), "tn_lo", "tlT"])
            P.fence(["tn_lo", "tlT", ("tn_f", 1), ("w", 3)] + [("m", d) for d in range(KC)] + [("cz", j) for j in range(4)] + [("acc", j) for j in range(4)])
            P.fence(["gbc", "wsT", "wsT_f", "sel", ("w", 4)])
            enable_slot3()
            for j in range(NT):
                P.dma("pool", xn_bf[j % 2], tnu_d, f"gt{j % 2}", indirect=True, out_offset=None, in_offset=IOA(ap=inv_sb[:, j, 0:1], axis=0),
                      reads=["inv"] + [("tnu", t) for t in range(NT)], writes=[("xn_bf", j % 2)])
                transpose_tile(xn_bf[j % 2], xnT, j, ("xn_bf", j % 2), ("xnT", j), evac_alt=j)
            for j in range(NT):
                P.dma("pool", gate_s[:, j, :], gu_d, f"gg{j}", indirect=True, out_offset=None, in_offset=IOA(ap=inv_sb[:, j, 0:1], axis=0),
                      reads=["inv", "gu"], writes=[("gate_s", j)])
            for j in range(NT):
                P.dma("pool", h_tm[:, j, :], hu_d, f"gh{j}", indirect=True, out_offset=None, in_offset=IOA(ap=inv_sb[:, j, 0:1], axis=0),
                      reads=["inv"] + [("hu", t) for t in range(NT)], writes=[("h", j)])
            dump("tTs", xnT[:], [128, KC, TOK], BF16, xnT_keys)
            dump("gate_s", gate_s[:], [128, NT, NE], F32, [("gate_s", j) for j in range(NT)])

        if stop_after >= 6:
            P.fence(["junk", ("xn_bf", 0), ("xn_bf", 1), ("tn_f", 0), ("tn_f", 1), "tn_lo", "tlT", ("sg", 0), ("sg", 1)]
                    + [("hid", j) for j in range(NT)] + [("hid_tm", j) for j in range(NT)])
            ENG3 = ("pe", "act", "dve")
            hT = hidT1

            def load_flags(stream, gi):
                for en in ENG3:
                    def ld(engine, en=en, gi=gi, stream=stream):
                        if en not in P.regs:
                            P.regs[en] = [engine.alloc_register(f"flag_{en}_{j}") for j in range(2 * NT)]
                        ins = None
                        for j in range(NT):
                            ins = engine.reg_load(P.regs[en][stream * NT + j], F_i[0:1, gi * 8 + j:gi * 8 + j + 1])
                        return ins
                    P.add(en, ld, reads=["flags"])

            def pass_A(ex):
                wg_v, kg = w_acquire()
                wu_v, ku = w_acquire()
                for j in range(NT):
                    P.cond_begin(ENG3, j)
                    bg = banks(1)
                    bu = banks(1)

                    def g(e, j=j, bg=bg, bu=bu, wg_v=wg_v, wu_v=wu_v):
                        ins = None
                        for k in range(KC):
                            lhs = xnT[:, k, j * 128:(j + 1) * 128]
                            e.matmul(ps[:, bg, :], lhsT=lhs, rhs=wg_v[:, k, :], start=(k == 0), stop=(k == KC - 1))
                            ins = e.matmul(ps[:, bu, :], lhsT=lhs, rhs=wu_v[:, k, :], start=(k == 0), stop=(k == KC - 1))
                        return ins
                    P.add("pe", g, reads=[kg, ku, ("xnT", j)], writes=[PS(bg), PS(bu)])
                    P.add("act", lambda e, j=j, bg=bg: e.activation(out=sg[j % 2], in_=ps[:, bg, :], func=AF.Silu),
                          reads=[PS(bg)], writes=[("sg", j % 2)])
                    P.add("dve", lambda e, j=j, bu=bu, ex=ex: e.scalar_tensor_tensor(out=hid_tm[:, j, :], in0=ps[:, bu, :], scalar=gate_s[:, j, ex:ex + 1],
                                                                                   in1=sg[j % 2], op0=ALU.mult, op1=ALU.mult),
                          reads=[PS(bu), ("sg", j % 2), ("gate_s", j)], writes=[("hid_tm", j)])
                    P.cond_end(ENG3)
                w_release()
                w_release()

            def pass_T(ex):
                for j in range(NT):
                    P.cond_begin(("pe", "act"), NT + j)
                    bt = banks(1)
                    pst = ps[:, bt, :].bitcast(BF16)

                    def trg(e, j=j, pst=pst):
                        ins = None
                        for k in range(4):
                            ins = e.transpose(out=pst[:, k * 128:(k + 1) * 128], in_=hid_tm[:, j, k * 128:(k + 1) * 128], identity=ident[:])
                        return ins
                    P.add("pe", trg, reads=[("hid_tm", j), "ident"], writes=[PS(bt)])
                    P.add("act", lambda e, j=j, pst=pst: e.copy(out=hT[:, 0:4, j * 128:(j + 1) * 128],
                                                               in_=pst[:, 0:512].rearrange("p (k c) -> p k c", c=128)),
                          reads=[PS(bt)], writes=[("hid", j)])
                    P.cond_end(("pe", "act"))

            def pass_B(ex):
                wd_v, kd = w_acquire()
                for j in range(NT):
                    P.cond_begin(("pe", "dve"), NT + j)
                    b0 = banks(4)

                    def g(e, j=j, b0=b0, wd_v=wd_v):
                        ins = None
                        for k in range(4):
                            lhs = hT[:, k, j * 128:(j + 1) * 128]
                            for n in range(4):
                                ins = e.matmul(ps[:, b0 + n, :], lhsT=lhs, rhs=wd_v[:, k, n * 512:(n + 1) * 512], start=(k == 0), stop=(k == 3))
                        return ins
                    P.add("pe", g, reads=[kd, ("hid", j)], writes=[PS(b0 + i) for i in range(4)])
                    P.add("dve", lambda e, j=j, b0=b0: e.tensor_tensor(
                        out=h_tm[:, j, :], in0=ps[:, b0:b0 + 4, :].rearrange("p a b -> p (a b)"), in1=h_tm[:, j, :], op=ALU.add),
                        reads=[PS(b0 + i) for i in range(4)] + [("h", j)], writes=[("h", j)])
                    P.cond_end(("pe", "dve"))
                w_release()

            load_flags(0, 0)
            pass_A(0)
            for ex in range(NE):
                if ex % 4 == 0:
                    load_flags(1, ex // 4)
                pass_T(ex)
                if ex + 1 < NE:
                    if (ex + 1) % 4 == 0:
                        load_flags(0, (ex + 1) // 4)
                    pass_A(ex + 1)
                pass_B(ex)
            dump("h2s", h_tm, [128, NT, D], F32, R1_keys)

        if stop_after >= 7:
            P.fence([("w", 4), "gbc"])
            P.dma("sp", gbc[:], gfin_d, "c_gbc", writes=["gbc"])
            P.fence(["junk", ("sg", 0), ("sg", 1)] + [("hid", j) for j in range(NT)] + [("hid_tm", j) for j in range(NT)])
            rms_stats(lambda t: h_tm[:, t, :], 2, 0)
            for t in range(NT):
                if t + 1 < NT:
                    rms_stats(lambda t: h_tm[:, t, :], 2, t + 1)
                P.add("dve", lambda e, t=t: e.scalar_tensor_tensor(out=h_tm[:, t, :], in0=h_tm[:, t, :], scalar=rstd[:, 2, t:t + 1], in1=gbc[:],
                                                                   op0=ALU.mult, op1=ALU.mult),
                      reads=[("h", t), ("rstd", 2, t), "gbc"], writes=[("h", t)])
                P.dma("pool", out_d, h_tm[:, t, :], f"o{t}", indirect=True, out_offset=IOA(ap=inv_sb[:, t, 0:1], axis=0), in_offset=None,
                      reads=[("h", t), "inv"], writes=[("out", t)])
        P.add("sp", None, reads=[("out", t) for t in range(NT)] + [("dbg", n) for n in dbg_out])
        P.emit()
    return nc, dbg_out


def make_in_maps(x, norm_mix_g, w_in, conv_w, sgu_ln_g, sgu_ln_b, sgu_w_s, sgu_b_s,
                 w_up_conv, w_up_sgu, w_out, norm_ffn_g, w_router_group, b_router_group,
                 w_router_expert, b_router_expert, w_exp_gate, w_exp_up, w_exp_down, norm_final_g, ncores=NCORES):
    f = lambda a: np.ascontiguousarray(np.asarray(a, dtype=np.float32))
    x2 = f(x).reshape(SEQ, D)
    bcast = lambda v, n: np.ascontiguousarray(np.broadcast_to(f(v).reshape(1, n), (128, n)))
    wr = np.concatenate([f(w_router_group)[0], f(w_router_expert)[0]], axis=1)
    br = np.concatenate([f(b_router_group)[0], f(b_router_expert)[0]], axis=0)
    sel = np.zeros((8, 1024), dtype=ml_dtypes.bfloat16)
    for h in range(8):
        sel[h, h * 128:(h + 1) * 128] = 1
    shared = {
        "w_in": f(w_in)[0], "w_up_conv": f(w_up_conv)[0], "w_up_sgu": f(w_up_sgu)[0], "w_out": f(w_out)[0],
        "w_exp_gate": f(w_exp_gate)[0], "w_exp_up": f(w_exp_up)[0], "w_exp_down": f(w_exp_down)[0],
        "gmix_bc": bcast(norm_mix_g, D), "gffn_bc": bcast(norm_ffn_g, D), "gfin_bc": bcast(norm_final_g, D),
        "lng_bc": bcast(sgu_ln_g, SW), "lnb_bc": bcast(sgu_ln_b, SW),
        "cw": np.ascontiguousarray(f(conv_w)[0].reshape(3, 8, 128).transpose(2, 1, 0)),
        "wsT": np.ascontiguousarray(f(sgu_w_s)[0].transpose(2, 0, 1)),
        "bs": f(sgu_b_s)[0],
        "wr": np.ascontiguousarray(wr.reshape(KC, 128, 20).transpose(1, 0, 2)),
        "br_bc": bcast(br, 20),
        "ident": np.eye(128, dtype=ml_dtypes.bfloat16),
        "sel": sel,
        "tri": np.concatenate([np.ones((128, 128), np.float32), np.triu(np.ones((128, 128), np.float32), 1)], axis=1).astype(ml_dtypes.bfloat16),
        "thr": np.ascontiguousarray(np.broadcast_to(np.concatenate([128.0 * np.arange(8), 128.0 * (np.arange(8) + 1)]).astype(np.float32), (128, 16))),
        "tokid": np.ascontiguousarray(np.broadcast_to((np.arange(NT)[None, :, None] * 128 + np.arange(128)[:, None, None]).astype(np.int32), (128, NT, 16))),
    }
    maps = []
    for c in range(ncores):
        m = dict(shared)
        m["x"] = np.ascontiguousarray(x2[c * TOK:(c + 1) * TOK])
        m["xh"] = np.ascontiguousarray(x2[c * TOK - 2:c * TOK]) if c > 0 else np.zeros((2, D), np.float32)
        maps.append(m)
    return maps


_NC_CACHE = {}


def kernel(**inputs):
    if "nc" not in _NC_CACHE:
        _NC_CACHE["nc"] = build_program()[0]
    nc = _NC_CACHE["nc"]
    in_maps = make_in_maps(**inputs)
    res = run_bass_kernel_spmd(nc, in_maps, core_ids=list(range(NCORES)))
    out = np.concatenate([np.asarray(r["out"]) for r in res.results], axis=0)
    return out.reshape(1, SEQ, D).astype(np.float32)
```

```python
from contextlib import ExitStack
import numpy as np
import ml_dtypes
import concourse.bass as bass
import concourse.mybir as mybir
from concourse.bass_utils import run_bass_kernel_spmd

F32 = mybir.dt.float32
BF16 = mybir.dt.bfloat16
AF = mybir.ActivationFunctionType
ALU = mybir.AluOpType
AX = mybir.AxisListType

NCORES = 8
D = 2048
SEQ = 8192
TOK = SEQ // NCORES
NT = TOK // 128
KC = D // 128
CW = 1024
SW = 1024
NE = 16
FE = 512
EPS = 1e-6
NSLOT = 3
INFLIGHT = 2

ENGS = ("pe", "act", "dve", "pool", "sp")


class Op:
    __slots__ = ("eng", "fn", "deps", "is_dma", "sem", "val", "signal", "idx", "name", "_pending")

    def __init__(self, eng, fn, name=""):
        self.eng = eng
        self.fn = fn
        self.deps = []
        self.is_dma = False
        self.sem = None
        self.val = 0
        self.signal = False
        self.idx = -1
        self.name = name


class Prog:
    def __init__(self, nc, stack):
        self.nc = nc
        self.stack = stack
        self.ops = []
        self.eng_ops = {e: [] for e in ENGS}
        self.last_writer = {}
        self.readers = {}
        self.esem = {e: stack.enter_context(nc.semaphore("prog_" + e)) for e in ENGS}
        self.dma_sems = {}
        self.dma_counts = {}
        self.regs = {}
        self.in_cond = False

    def cond_begin(self, engs, flag_idx):
        self.in_cond = True
        for e in engs:
            self.eng_ops[e].append(("begin", flag_idx))

    def cond_end(self, engs):
        self.in_cond = False
        for e in engs:
            self.eng_ops[e].append(("end",))

    def _track(self, op, reads, writes):
        qkey = lambda o: ("d", o.sem) if o.is_dma else ("e", o.eng)
        deps = []
        for k in reads:
            deps.extend(self.last_writer.get(k, {}).values())
        for k in writes:
            deps.extend(self.readers.get(k, {}).values())
            deps.extend(self.last_writer.get(k, {}).values())
        op._pending = (list(reads), list(writes))
        flat = []
        for d in deps:
            if d is op:
                continue
            if d.eng is None:
                flat.extend(d.deps)
            else:
                flat.append(d)
        best = {}
        for d in flat:
            key = qkey(d)
            cur = best.get(key)
            if cur is None or d.idx > cur.idx:
                best[key] = d
        op.deps = list(best.values())

    def _commit(self, op):
        reads, writes = op._pending
        if op.eng is None:
            k0 = ("v", op.idx)
        else:
            k0 = ("d", op.sem) if op.is_dma else ("e", op.eng)
        for k in reads:
            self.readers.setdefault(k, {})[k0] = op
        for k in writes:
            if self.in_cond and op.eng is not None:
                self.last_writer.setdefault(k, {})[k0] = op
            else:
                self.last_writer[k] = {k0: op}
                self.readers[k] = {}

    def add(self, eng, fn, reads=(), writes=(), name="", _defer=False):
        op = Op(eng, fn, name)
        op.idx = len(self.ops)
        self._track(op, reads, writes)
        if not _defer:
            self._commit(op)
        self.ops.append(op)
        self.eng_ops[eng].append(op)
        return op

    def dma(self, eng, out, in_, semname, reads=(), writes=(), name="", **kw):
        if semname not in self.dma_sems:
            self.dma_sems[semname] = self.stack.enter_context(self.nc.semaphore("d_" + semname))
            self.dma_counts[semname] = 0
        if kw.pop("indirect", False):
            fn = lambda e: e.indirect_dma_start(out=out, in_=in_, **kw)
        else:
            fn = lambda e: e.dma_start(out=out, in_=in_, **kw)
        op = self.add(eng, fn, reads, writes, name, _defer=True)
        op.is_dma = True
        op.sem = semname
        self._commit(op)
        self.dma_counts[semname] += 1
        op.val = 16 * self.dma_counts[semname]
        return op

    def fence(self, keys, eng=None):
        keys = list(keys)
        op = Op(None, None, "fence")
        op.idx = len(self.ops)
        self._track(op, keys, keys)
        self._commit(op)
        return op

    def emit(self):
        nc = self.nc
        for op in self.ops:
            for d in op.deps:
                if not d.is_dma:
                    d.signal = True
        cnt = {e: 0 for e in ENGS}
        for op in self.ops:
            if op.is_dma:
                continue
            if op.signal:
                cnt[op.eng] += 1
                op.val = cnt[op.eng]
        handles = {"pe": "tensor", "act": "scalar", "dve": "vector", "pool": "gpsimd", "sp": "sync"}
        with nc.Block() as block:
            for e in ENGS:
                ops = self.eng_ops[e]
                if not ops:
                    continue

                def body(engine, ops=ops, e=e):
                    waited = {}
                    saved = None
                    guard = None
                    n_sig = 0
                    for op in ops:
                        if isinstance(op, tuple):
                            if op[0] == "begin":
                                saved = dict(waited)
                                n_sig = 0
                                guard = engine.If_eq(self.regs[e][op[1]], 1)
                                guard.__enter__()
                            else:
                                guard.__exit__(None, None, None)
                                if n_sig:
                                    with engine.Else():
                                        engine.drain(fusable=False).then_inc(self.esem[e], n_sig)
                                waited = saved
                                guard = None
                            continue
                        for d in op.deps:
                            if d.is_dma:
                                key = ("d", d.sem)
                                sem = self.dma_sems[d.sem]
                            else:
                                key = ("e", d.eng)
                                sem = self.esem[d.eng]
                            if waited.get(key, 0) >= d.val:
                                continue
                            waited[key] = d.val
                            engine.wait_ge(sem, d.val)
                        ins = op.fn(engine) if op.fn is not None else None
                        if op.is_dma:
                            ins.then_inc(self.dma_sems[op.sem], 16)
                        elif op.signal:
                            if ins is None:
                                ins = engine.drain(fusable=False)
                            ins.then_inc(self.esem[e], 1)
                            if guard is not None:
                                n_sig += 1

                getattr(block, handles[e])(body)


def build_program(debug=(), stop_after=99):
    nc = bass.Bass("TRN2", target_bir_lowering=False)
    dram_in = lambda n, s, d=F32: nc.dram_tensor(n, list(s), d, kind="ExternalInput").ap()
    x_d = dram_in("x", [TOK, D])
    xh_d = dram_in("xh", [2, D])
    w_in_d = dram_in("w_in", [D, 9216])
    w_upc_d = dram_in("w_up_conv", [CW, D])
    w_ups_d = dram_in("w_up_sgu", [SW, D])
    w_out_d = dram_in("w_out", [D, D])
    wg_d = dram_in("w_exp_gate", [NE, D, FE])
    wu_d = dram_in("w_exp_up", [NE, D, FE])
    wd_d = dram_in("w_exp_down", [NE, FE, D])
    gmix_d = dram_in("gmix_bc", [128, D])
    gffn_d = dram_in("gffn_bc", [128, D])
    gfin_d = dram_in("gfin_bc", [128, D])
    lng_d = dram_in("lng_bc", [128, SW])
    lnb_d = dram_in("lnb_bc", [128, SW])
    cw_d = dram_in("cw", [128, 8, 3])
    wsT_d = dram_in("wsT", [128, 8, 128])
    bs_d = dram_in("bs", [8, 128])
    wr_d = dram_in("wr", [128, KC, 20])
    br_d = dram_in("br_bc", [128, 20])
    ident_d = dram_in("ident", [128, 128], BF16)
    sel_d = dram_in("sel", [8, 1024], BF16)
    tri_d = dram_in("tri", [128, 256], BF16)
    thr_d = dram_in("thr", [128, 16])
    tokid_d = dram_in("tokid", [128, NT, 16], mybir.dt.int32)
    out_d = nc.dram_tensor("out", [TOK, D], F32, kind="ExternalOutput").ap()
    hu_d = nc.dram_tensor("hu_scr", [TOK, D], F32, kind="Internal").ap()
    tnu_d = nc.dram_tensor("tnu_scr", [TOK, D], BF16, kind="Internal").ap()
    gu_d = nc.dram_tensor("gu_scr", [TOK, NE], F32, kind="Internal").ap()
    iv_d = nc.dram_tensor("iv_scr", [TOK, 16], mybir.dt.int32, kind="Internal").ap()
    dbg_out = {}

    with ExitStack() as st:
        P = Prog(nc, st)
        sb = lambda n, s, d: st.enter_context(nc.sbuf_tensor(n, list(s), d))

        R1 = sb("R1", [128, 65536], mybir.dt.uint8)
        R2 = sb("R2", [128, 36864], mybir.dt.uint8)
        xnT = sb("xnT", [128, KC, TOK], BF16)
        wring = [sb(f"wring{i}", [128, 8192], BF16) for i in range(NSLOT)]
        R3 = sb("R3", [128, 16384], mybir.dt.uint8)
        ident = sb("ident_sb", [128, 128], BF16)
        cwt = sb("cw_sb", [128, 8, 3], F32)
        bs_f = sb("bs_f", [8, 128], F32)
        bs_hi = sb("bs_hi", [8, 128], BF16)
        bs_lo = sb("bs_lo", [8, 128], BF16)
        wr_f = sb("wr_f", [128, KC, 20], F32)
        wr_hi = sb("wr_hi", [128, KC, 20], BF16)
        wr_lo = sb("wr_lo", [128, KC, 20], BF16)
        br = sb("br_sb", [128, 20], F32)
        xnTh = sb("xnTh", [128, KC, 2], BF16)
        ss = sb("ss", [128, 4, NT], F32)
        rstd = sb("rstd", [128, 4, NT], F32)
        mv = sb("mv", [128, NT, 2], F32)
        bnst = sb("bnst", [128, 2, 6], F32)
        lnr = sb("lnr", [128, NT], F32)
        rt = sb("rt", [128, 704], F32)
        gate = sb("gate", [128, NT, NE], F32)
        gate_s = sb("gate_s", [128, NT, NE], F32)
        tri = sb("tri_sb", [128, 256], BF16)
        thr = sb("thr_sb", [128, 16], F32)
        tokid = sb("tokid_sb", [128, NT, 16], mybir.dt.int32)
        inv_sb = sb("inv_sb", [128, NT, 16], mybir.dt.int32)
        dest_i = sb("dest_i", [128, NT], mybir.dt.int32)
        F_i = sb("F_i", [128, 32], mybir.dt.int32)
        ohg_bf = sb("ohg_bf", [128, NT, 4], BF16)

        def view(region, off, shape, dt):
            esz = {F32: 4, BF16: 2}[dt]
            n = int(np.prod(shape[1:]))
            ap = region[:, off:off + n * esz].bitcast(dt)
            if len(shape) == 3:
                ap = ap.rearrange("p (a b) -> p a b", b=shape[2])
            return ap

        gbc = view(R3, 0, [128, D], F32)
        wsT_f = view(R3, 8192, [128, 8, 128], F32)
        wsT = view(R3, 12288, [128, 8, 128], BF16)
        sel = view(R3, 14336, [128, 1024], BF16)[0:8, :]
        h_tm = view(R1, 0, [128, NT, D], F32)
        yA = view(R1, 0, [128, 8, TOK], BF16)
        yB = view(R1, 16384, [128, 8, TOK], BF16)
        v_tok = view(R1, 32768, [128, NT, SW], BF16)
        vg = view(R1, 49152, [128, 4, SW], F32)
        xn_bf = [view(R2, i * 4096, [128, D], BF16) for i in range(2)]
        junk = view(R2, 8192, [128, D], BF16)
        tn_f = view(R2, 12288, [128, D], F32)
        tn_f2 = view(R2, 28672, [128, D], F32)
        tn_lo = view(R2, 20480, [128, D], BF16)
        tlT = view(R2, 24576, [128, KC, 128], BF16)
        xh = view(R2, 12288, [128, D], F32)[0:2, :]
        xh_bf = view(R2, 20480, [128, D], BF16)[0:2, :]
        cz = view(R2, 0, [128, 4, TOK + 2], F32)
        acc = view(R2, 16448, [128, 4, TOK], F32)
        m_sb = view(R2, 0, [128, KC, TOK], BF16)
        hidT1 = view(R2, 0, [128, 4, TOK], BF16)
        hid_tm = view(R2, 8192, [128, NT, 512], BF16)
        sg = [view(R2, 16384 + i * 2048, [128, 512], F32) for i in range(2)]
        wring.append(R2[:, 20480:36864].bitcast(BF16))
        wring.append(R3[:, :].bitcast(BF16))
        sig_t = [view(R1, 32768 + i * 2048, [128, 512], F32) for i in range(4)]

        ps = st.enter_context(nc.psum_tensor("ps", [128, 8, 512], F32))
        bank_ptr = [0]

        def banks(n):
            b = bank_ptr[0]
            if b % n:
                b += n - b % n
            if b + n > 8:
                b = 0
            bank_ptr[0] = (b + n) % 8
            return b

        PS = lambda b: ("ps", b)

        def dump(name, ap, shape, dt, reads):
            if name not in debug:
                return
            t = nc.dram_tensor("dbg_" + name, list(shape), dt, kind="ExternalOutput").ap()
            dbg_out[name] = t
            P.dma("sp", t, ap, "dbg_" + name, reads=reads, writes=[("dbg", name)])

        w_in_v = w_in_d.rearrange("(kc p) c -> p kc c", p=128)
        upc_v = w_upc_d.rearrange("(kc p) c -> p kc c", p=128)
        ups_v = w_ups_d.rearrange("(kc p) c -> p kc c", p=128)
        wout_v = w_out_d.rearrange("(kc p) c -> p kc c", p=128)
        pieces = []

        def piece_in(c0):
            pieces.append((w_in_v[:, :, c0:c0 + 512], KC, 512))

        for half in range(2):
            piece_in(1024 + half * 512)
            piece_in(2048 + half * 512)
            piece_in(0 + half * 512)
        piece_in(4096); piece_in(4608)
        piece_in(3072); piece_in(3584)
        for dg in range(4):
            piece_in(5120 + dg * 512)
            pieces.append((upc_v[:, :, dg * 512:(dg + 1) * 512], 8, 512))
            piece_in(7168 + dg * 512)
            pieces.append((ups_v[:, :, dg * 512:(dg + 1) * 512], 8, 512))
        for n in range(4):
            pieces.append((wout_v[:, :, n * 512:(n + 1) * 512], KC, 512))
        for e in range(NE):
            pieces.append((wg_d[e].rearrange("(kc p) f -> p kc f", p=128), KC, 512))
            pieces.append((wu_d[e].rearrange("(kc p) f -> p kc f", p=128), KC, 512))
            pieces.append((wd_d[e].rearrange("(kc p) f -> p kc f", p=128), 4, 2048))
        issued = [0]
        cur = [0]
        released = [0]
        n_mixer = len(pieces) - 3 * NE
        piece_slot = [i % 3 if i < n_mixer else (i - n_mixer) % 5 for i in range(len(pieces))]
        slot_owner = {0: None, 1: None, 2: None, 3: -1, 4: -1}

        def slot_view(i):
            _, kc, cols = pieces[i]
            return wring[piece_slot[i]][:, 0:kc * cols].rearrange("p (kc c) -> p kc c", c=cols)

        def try_issue():
            while issued[0] < len(pieces):
                i = issued[0]
                sl = piece_slot[i]
                if slot_owner[sl] is not None:
                    break
                slot_owner[sl] = i
                issued[0] += 1
                P.dma("pool", slot_view(i), pieces[i][0], f"w{sl}", writes=[("w", sl), ("wtok", i % INFLIGHT)], name=f"wpiece{i}")

        def w_acquire():
            i = cur[0]
            cur[0] += 1
            assert i < issued[0], "weight piece not yet issued (ring too small for this access pattern)"
            return slot_view(i), ("w", piece_slot[i])

        def w_release():
            i = released[0]
            released[0] += 1
            slot_owner[piece_slot[i]] = None
            try_issue()

        def enable_slot3():
            slot_owner[3] = None
            slot_owner[4] = None
            try_issue()

        issue_next = try_issue

        P.dma("sp", ident[:], ident_d, "c_ident", writes=["ident"])
        P.dma("act", gbc[:], gmix_d, "c_gbc", writes=["gbc"])
        for t in range(NT):
            P.dma("sp" if t % 2 == 0 else "act", h_tm[:, t, :], x_d[t * 128:(t + 1) * 128, :], f"x{t}", writes=[("h", t)])
        P.dma("sp", xh[:], xh_d, "c_xh", writes=["xh"])
        P.dma("act", cwt[:], cw_d, "c_cw", writes=["cw"])
        P.dma("sp", wsT_f[:], wsT_d, "c_ws", writes=["wsT_f"])
        P.dma("act", bs_f[:], bs_d, "c_bs", writes=["bs_f"])
        P.dma("sp", sel[:], sel_d, "c_sel", writes=["sel"])
        P.dma("act", wr_f[:], wr_d, "c_wr", writes=["wr_f"])
        P.dma("sp", br[:], br_d, "c_br", writes=["br"])
        P.dma("act", tri[:], tri_d, "c_tri", writes=["tri"])
        P.dma("sp", thr[:], thr_d, "c_thr", writes=["thr"])
        P.dma("act", tokid[:], tokid_d, "c_tokid", writes=["tokid"])
        try_issue()

        P.add("dve", lambda e: e.memset(ss[:], 0.0), writes=["ss"])
        def rms_stats(src_tile, nrm, t):
            if True:
                P.add("act", lambda e, t=t: e.activation(out=junk[:], in_=src_tile(t), func=AF.Square,
                                                         accum_out=ss[:, nrm, t:t + 1]),
                      reads=[("h", t), "ss"], writes=["junk", ("ss", nrm, t)])
                P.add("act", lambda e, t=t: e.activation(out=rstd[:, nrm, t:t + 1], in_=ss[:, nrm, t:t + 1], func=AF.Sqrt,
                                                         scale=1.0 / D, bias=eps_t[:, 0:1]),
                      reads=[("ss", nrm, t), "eps"], writes=[("rstd", nrm, t)])
                P.add("dve", lambda e, t=t: e.reciprocal(out=rstd[:, nrm, t:t + 1], in_=rstd[:, nrm, t:t + 1]),
                      reads=[("rstd", nrm, t)], writes=[("rstd", nrm, t)])

        eps_t = sb("eps_t", [128, 1], F32)
        P.add("dve", lambda e: e.memset(eps_t[:], EPS), writes=["eps"])

        def transpose_tile(src_bf, dst, t, src_key, dst_key, rows=128, width=128, evac_alt=0):
            for hb in range(2):
                b = banks(1)
                pst = ps[:, b, :].bitcast(BF16)

                def trg(e, hb=hb, pst=pst):
                    ins = None
                    for j in range(8):
                        k = hb * 8 + j
                        ins = e.transpose(out=pst[:, j * width:(j + 1) * width],
                                          in_=src_bf[0:rows, k * 128:(k + 1) * 128],
                                          identity=ident[0:rows, 0:rows])
                    return ins
                P.add("pe", trg, reads=[src_key, "ident"], writes=[PS(b)])
                src_v = pst[:, 0:8 * width].rearrange("p (j c) -> p j c", c=width)
                dst_v = dst[:, hb * 8:(hb + 1) * 8, t * width:(t + 1) * width]
                if (hb + evac_alt) % 2 == 0:
                    P.add("act", lambda e, s=src_v, d=dst_v: e.copy(out=d, in_=s), reads=[PS(b)], writes=[dst_key])
                else:
                    P.add("dve", lambda e, s=src_v, d=dst_v: e.tensor_copy(out=d, in_=s), reads=[PS(b)], writes=[dst_key])

        def mm_A(wv, wkey, nk, col0, rhs_fn, rhs_keys, halo=None):
            b0 = banks(2)
            bh = banks(1) if halo is not None else None

            def g(e):
                ins = None
                for k in range(nk):
                    lhs = wv[:, k, col0:col0 + 128]
                    for n2 in range(2):
                        ins = e.matmul(ps[:, b0 + n2, :], lhsT=lhs, rhs=rhs_fn(k, n2), start=(k == 0), stop=(k == nk - 1))
                    if halo is not None:
                        ins = e.matmul(ps[:, bh, 0:2], lhsT=lhs, rhs=halo(k), start=(k == 0), stop=(k == nk - 1))
                return ins
            wr = [PS(b0), PS(b0 + 1)] + ([PS(bh)] if halo is not None else [])
            P.add("pe", g, reads=[wkey] + list(rhs_keys), writes=wr)
            return b0, bh

        xn_rhs = lambda k, n2: xnT[:, k, n2 * 512:(n2 + 1) * 512]
        xnT_keys = [("xnT", t) for t in range(NT)]

        rms_stats(lambda t: h_tm[:, t, :], 0, 0)
        for t in range(NT):
            if t + 1 < NT:
                rms_stats(lambda t: h_tm[:, t, :], 0, t + 1)
            xb = xn_bf[t % 2]
            P.add("dve", lambda e, t=t, xb=xb: e.scalar_tensor_tensor(out=xb, in0=h_tm[:, t, :], scalar=rstd[:, 0, t:t + 1],
                                                                      in1=gbc[:], op0=ALU.mult, op1=ALU.mult),
                  reads=[("h", t), ("rstd", 0, t), "gbc"], writes=[("xn_bf", t % 2)])
            transpose_tile(xb, xnT, t, ("xn_bf", t % 2), ("xnT", t), evac_alt=t)
        P.add("act", lambda e: e.activation(out=junk[0:2, :], in_=xh[:], func=AF.Square, accum_out=ss[0:2, 3, 0:1]),
              reads=["xh", "ss"], writes=["junk", "ssh"])
        P.add("act", lambda e: e.activation(out=rstd[0:2, 3, 0:1], in_=ss[0:2, 3, 0:1], func=AF.Sqrt, scale=1.0 / D, bias=eps_t[0:2, 0:1]),
              reads=["ssh", "eps"], writes=["rstdh"])
        P.add("dve", lambda e: e.reciprocal(out=rstd[0:2, 3, 0:1], in_=rstd[0:2, 3, 0:1]), reads=["rstdh"], writes=["rstdh"])
        P.add("dve", lambda e: e.scalar_tensor_tensor(out=xh_bf[:], in0=xh[:], scalar=rstd[0:2, 3, 0:1], in1=gbc[0:2, :],
                                                      op0=ALU.mult, op1=ALU.mult),
              reads=["xh", "rstdh", "gbc"], writes=["xh_bf"])
        transpose_tile(xh_bf, xnTh, 0, "xh_bf", "xnTh", rows=2, width=2)
        P.add("dve", lambda e: e.tensor_copy(out=wsT[:], in_=wsT_f[:]), reads=["wsT_f"], writes=["wsT"])
        P.add("dve", lambda e: e.memset(wsT[64:128, :, 0:64], 0.0), writes=["wsT"])
        P.add("dve", lambda e: e.tensor_copy(out=bs_hi[:], in_=bs_f[:]), reads=["bs_f"], writes=["bs_hi"])
        P.add("dve", lambda e: e.tensor_tensor(out=bs_lo[:], in0=bs_f[:], in1=bs_hi[:], op=ALU.subtract),
              reads=["bs_f", "bs_hi"], writes=["bs_lo"])
        P.add("dve", lambda e: e.tensor_copy(out=wr_hi[:], in_=wr_f[:]), reads=["wr_f"], writes=["wr_hi"])
        P.add("dve", lambda e: e.tensor_tensor(out=wr_lo[:], in0=wr_f[:], in1=wr_hi[:], op=ALU.subtract),
              reads=["wr_f", "wr_hi"], writes=["wr_lo"])

        dump("xnT", xnT[:], [128, KC, TOK], BF16, xnT_keys)
        dump("xnTh", xnTh[:], [128, KC, 2], BF16, ["xnTh"])

        R1_keys = [("h", t) for t in range(NT)]
        P.fence(R1_keys + ["yA"] + [("yB", j) for j in range(8)] + [("v_tok", t) for t in range(NT)] + [("vg", q) for q in range(4)])
        P.fence(["junk", ("xn_bf", 0), ("xn_bf", 1), "xh", "xh_bf"] + [("cz", j) for j in range(4)] + [("acc", j) for j in range(4)])
        P.dma("sp", gbc[:, 0:SW], lng_d, "c_gbc", reads=[], writes=["gbc"])
        P.dma("sp", gbc[:, SW:2 * SW], lnb_d, "c_gbc", reads=[], writes=["gbc"])

        if stop_after >= 1:
            halo_rhs = lambda k: xnTh[:, k, :]
            for half in range(2):
                wv, wk = w_acquire()
                for jj in range(4):
                    b0, bh = mm_A(wv, wk, KC, jj * 128, xn_rhs, xnT_keys + ["xnTh"], halo=halo_rhs)
                    for n2 in range(2):
                        P.add("act", lambda e, jj=jj, n2=n2, b0=b0: e.copy(out=cz[:, jj, 2 + n2 * 512:2 + (n2 + 1) * 512], in_=ps[:, b0 + n2, :]),
                              reads=[PS(b0 + n2)], writes=[("cz", jj)])
                    P.add("act", lambda e, jj=jj, bh=bh: e.copy(out=cz[:, jj, 0:2], in_=ps[:, bh, 0:2]), reads=[PS(bh)], writes=[("cz", jj)])
                w_release()
                wv, wk = w_acquire()
                for jj in range(4):
                    j = half * 4 + jj
                    b0, bh = mm_A(wv, wk, KC, jj * 128, xn_rhs, xnT_keys + ["xnTh"], halo=halo_rhs)
                    for n2 in range(2):
                        P.add("dve", lambda e, jj=jj, n2=n2, b0=b0: e.tensor_tensor(
                            out=cz[:, jj, 2 + n2 * 512:2 + (n2 + 1) * 512], in0=ps[:, b0 + n2, :],
                            in1=cz[:, jj, 2 + n2 * 512:2 + (n2 + 1) * 512], op=ALU.mult),
                            reads=[PS(b0 + n2), ("cz", jj)], writes=[("cz", jj)])
                    P.add("dve", lambda e, jj=jj, bh=bh: e.tensor_tensor(out=cz[:, jj, 0:2], in0=ps[:, bh, 0:2], in1=cz[:, jj, 0:2], op=ALU.mult),
                          reads=[PS(bh), ("cz", jj)], writes=[("cz", jj)])
                    P.add("act", lambda e, jj=jj, j=j: e.mul(out=acc[:, jj, :], in_=cz[:, jj, 2:TOK + 2], mul=cwt[:, j, 2:3]),
                          reads=[("cz", jj), "cw"], writes=[("acc", jj)])
                    P.add("dve", lambda e, jj=jj, j=j: e.scalar_tensor_tensor(out=acc[:, jj, :], in0=cz[:, jj, 1:TOK + 1], scalar=cwt[:, j, 1:2],
                                                                              in1=acc[:, jj, :], op0=ALU.mult, op1=ALU.add),
                          reads=[("cz", jj), "cw", ("acc", jj)], writes=[("acc", jj)])
                    P.add("dve", lambda e, jj=jj, j=j: e.scalar_tensor_tensor(out=acc[:, jj, :], in0=cz[:, jj, 0:TOK], scalar=cwt[:, j, 0:1],
                                                                              in1=acc[:, jj, :], op0=ALU.mult, op1=ALU.add),
                          reads=[("cz", jj), "cw", ("acc", jj)], writes=[("acc", jj)])
                w_release()
                wv, wk = w_acquire()
                for jj in range(4):
                    j = half * 4 + jj
                    b0, _ = mm_A(wv, wk, KC, jj * 128, xn_rhs, xnT_keys)
                    for n2 in range(2):
                        P.add("dve", lambda e, jj=jj, j=j, n2=n2, b0=b0: e.tensor_tensor(
                            out=yA[:, j, n2 * 512:(n2 + 1) * 512], in0=ps[:, b0 + n2, :],
                            in1=acc[:, jj, n2 * 512:(n2 + 1) * 512], op=ALU.mult),
                            reads=[PS(b0 + n2), ("acc", jj)], writes=["yA"])
                w_release()
            dump("yA", yA, [128, 8, TOK], BF16, ["yA"])

        if stop_after >= 2:
            wv0, wk0 = w_acquire()
            wv1, wk1 = w_acquire()
            for hq in range(2):
                for tq in range(4):
                    t = hq * 4 + tq
                    bv = banks(2)

                    def g(e, t=t, bv=bv):
                        ins = None
                        for k in range(KC):
                            lhs = xnT[:, k, t * 128:(t + 1) * 128]
                            e.matmul(ps[:, bv, :], lhsT=lhs, rhs=wv0[:, k, :], start=(k == 0), stop=(k == KC - 1))
                            ins = e.matmul(ps[:, bv + 1, :], lhsT=lhs, rhs=wv1[:, k, :], start=(k == 0), stop=(k == KC - 1))
                        return ins
                    P.add("pe", g, reads=[wk0, wk1, ("xnT", t)], writes=[PS(bv), PS(bv + 1)])
                    for pv in range(2):
                        b = bv + pv
                        P.add("act", lambda e, tq=tq, pv=pv, b=b: e.activation(out=vg[:, tq, pv * 512:(pv + 1) * 512], in_=ps[:, b, :],
                                                                              func=AF.Gelu_apprx_tanh),
                              reads=[PS(b)], writes=[("vg", tq)])
                        P.add("dve", lambda e, tq=tq, pv=pv: e.bn_stats(out=bnst[:, pv, :], in_=vg[:, tq, pv * 512:(pv + 1) * 512]),
                              reads=[("vg", tq)], writes=[("bnst", pv)])
                    P.add("dve", lambda e, t=t: e.bn_aggr(out=mv[:, t, :], in_=bnst[:].rearrange("p a b -> p (a b)")),
                          reads=[("bnst", 0), ("bnst", 1)], writes=[("mv", t)])
                sl = slice(hq * 4, hq * 4 + 4)
                P.add("act", lambda e, sl=sl: e.activation(out=lnr[:, sl], in_=mv[:, sl, 1], func=AF.Sqrt, bias=eps_t[:, 0:1]),
                      reads=[("mv", t) for t in range(hq * 4, hq * 4 + 4)] + ["eps"], writes=[("lnr", hq)])
                P.add("dve", lambda e, sl=sl: e.reciprocal(out=lnr[:, sl], in_=lnr[:, sl]), reads=[("lnr", hq)], writes=[("lnr", hq)])
                for tq in range(4):
                    t = hq * 4 + tq
                    P.add("dve", lambda e, t=t, tq=tq: e.tensor_scalar(out=vg[:, tq, :], in0=vg[:, tq, :], scalar1=mv[:, t, 0:1],
                                                                      scalar2=lnr[:, t:t + 1], op0=ALU.subtract, op1=ALU.mult),
                          reads=[("vg", tq), ("mv", t), ("lnr", hq)], writes=[("vg", tq)])
                    P.add("dve", lambda e, tq=tq: e.tensor_tensor(out=vg[:, tq, :], in0=vg[:, tq, :], in1=gbc[:, 0:SW], op=ALU.mult),
                          reads=[("vg", tq), "gbc"], writes=[("vg", tq)])
                    P.add("dve", lambda e, t=t, tq=tq: e.tensor_tensor(out=v_tok[:, t, :], in0=vg[:, tq, :], in1=gbc[:, SW:2 * SW], op=ALU.add),
                          reads=[("vg", tq), "gbc"], writes=[("v_tok", t)])
            w_release()
            w_release()
            for half in range(2):
                wv, wk = w_acquire()
                for jj in range(4):
                    j = half * 4 + jj
                    b0, _ = mm_A(wv, wk, KC, jj * 128, xn_rhs, xnT_keys)
                    for n2 in range(2):
                        P.add("act", lambda e, j=j, n2=n2, b0=b0: e.activation(out=yB[:, j, n2 * 512:(n2 + 1) * 512], in_=ps[:, b0 + n2, :],
                                                                              func=AF.Gelu_apprx_tanh),
                              reads=[PS(b0 + n2)], writes=[("yB", j)])
                w_release()
            for t in range(NT):
                for hg in range(2):
                    b = banks(1)

                    def g(e, t=t, hg=hg, b=b):
                        ins = None
                        for hh in range(4):
                            hd = hg * 4 + hh
                            o = ps[:, b, hh * 128:(hh + 1) * 128]
                            e.matmul(o, lhsT=v_tok[:, t, hd * 128:(hd + 1) * 128], rhs=wsT[:, hd, :], start=True, stop=False)
                            e.matmul(o, lhsT=sel[:, hd * 128:(hd + 1) * 128], rhs=bs_hi[:], start=False, stop=False)
                            ins = e.matmul(o, lhsT=sel[:, hd * 128:(hd + 1) * 128], rhs=bs_lo[:], start=False, stop=True)
                        return ins
                    P.add("pe", g, reads=[("v_tok", t), "wsT", "sel", "bs_hi", "bs_lo"], writes=[PS(b)])
                    P.add("dve", lambda e, t=t, hg=hg, b=b: e.tensor_tensor(
                        out=yB[:, hg * 4:(hg + 1) * 4, t * 128:(t + 1) * 128],
                        in0=ps[:, b, :].rearrange("p (h c) -> p h c", c=128),
                        in1=yB[:, hg * 4:(hg + 1) * 4, t * 128:(t + 1) * 128], op=ALU.mult),
                        reads=[PS(b)] + [("yB", hg * 4 + i) for i in range(4)], writes=[("yB", hg * 4 + i) for i in range(4)])
            dump("v_tok", v_tok, [128, NT, SW], BF16, [("v_tok", t) for t in range(NT)])
            dump("yB", yB, [128, 8, TOK], BF16, [("yB", j) for j in range(8)])

        if stop_after >= 3:
            P.fence(["cz", "acc"] + [("cz", j) for j in range(4)] + [("acc", j) for j in range(4)] + [("m", d) for d in range(KC)])
            P.fence([("v_tok", t) for t in range(NT)] + [("vg", q) for q in range(4)]
                    + [("sig", i) for i in range(4)] + [("tc", jj, n2) for jj in range(4) for n2 in range(2)])
            tc_t = {(jj, n2): view(R1, 40960 + (jj * 2 + n2) * 2048, [128, 512], F32) for jj in range(4) for n2 in range(2)}
            yA_rhs = lambda k, n2: yA[:, k, n2 * 512:(n2 + 1) * 512]
            yB_rhs = lambda k, n2: yB[:, k, n2 * 512:(n2 + 1) * 512]
            yB_keys = [("yB", j) for j in range(8)]
            for dg in range(4):
                for br_i in range(2):
                    wg_, kg_ = w_acquire()
                    wu_, ku_ = w_acquire()
                    for jj in range(4):
                        d = dg * 4 + jj
                        bg, _ = mm_A(wg_, kg_, KC, jj * 128, xn_rhs, xnT_keys)
                        for n2 in range(2):
                            si = (jj % 2) * 2 + n2
                            P.add("act", lambda e, bg=bg, n2=n2, si=si: e.activation(out=sig_t[si], in_=ps[:, bg + n2, :], func=AF.Sigmoid),
                                  reads=[PS(bg + n2)], writes=[("sig", si)])
                        if br_i == 0:
                            by, _ = mm_A(wu_, ku_, 8, jj * 128, yA_rhs, ["yA"])
                        else:
                            by, _ = mm_A(wu_, ku_, 8, jj * 128, yB_rhs, yB_keys)
                        for n2 in range(2):
                            si = (jj % 2) * 2 + n2
                            tt = tc_t[(jj, n2)]
                            if br_i == 0:
                                P.add("dve", lambda e, by=by, n2=n2, si=si, tt=tt: e.tensor_tensor(out=tt, in0=ps[:, by + n2, :], in1=sig_t[si], op=ALU.mult),
                                      reads=[PS(by + n2), ("sig", si)], writes=[("tc", jj, n2)])
                            else:
                                mo = m_sb[:, d, n2 * 512:(n2 + 1) * 512]
                                P.add("dve", lambda e, by=by, n2=n2, si=si: e.tensor_tensor(out=sig_t[si], in0=ps[:, by + n2, :], in1=sig_t[si], op=ALU.mult),
                                      reads=[PS(by + n2), ("sig", si)], writes=[("sig", si)])
                                P.add("dve", lambda e, si=si, tt=tt, mo=mo: e.tensor_tensor(out=mo, in0=sig_t[si], in1=tt, op=ALU.add),
                                      reads=[("sig", si), ("tc", jj, n2)], writes=[("m", d)])
                    w_release()
                    w_release()
            dump("m", m_sb, [128, KC, TOK], BF16, [("m", d) for d in range(KC)])

        if stop_after >= 4:
            P.fence(["yA"] + [("yB", j) for j in range(8)] + [("sig", i) for i in range(4)]
                    + [("tc", jj, n2) for jj in range(4) for n2 in range(2)] + R1_keys)
            for t in range(NT):
                P.dma("sp" if t % 2 == 0 else "act", h_tm[:, t, :], x_d[t * 128:(t + 1) * 128, :], f"x{t}", writes=[("h", t)])
            m_keys = [("m", d) for d in range(KC)]
            for n in range(4):
                wv, wk = w_acquire()
                for t in range(NT):
                    b = banks(1)

                    def g(e, t=t, wv=wv, b=b):
                        ins = None
                        for k in range(KC):
                            ins = e.matmul(ps[:, b, :], lhsT=m_sb[:, k, t * 128:(t + 1) * 128], rhs=wv[:, k, :], start=(k == 0), stop=(k == KC - 1))
                        return ins
                    P.add("pe", g, reads=[wk] + m_keys, writes=[PS(b)])
                    P.add("dve", lambda e, t=t, n=n, b=b: e.tensor_tensor(out=h_tm[:, t, n * 512:(n + 1) * 512], in0=ps[:, b, :],
                                                                         in1=h_tm[:, t, n * 512:(n + 1) * 512], op=ALU.add),
                          reads=[PS(b), ("h", t)], writes=[("h", t)])
                    if n == 3:
                        P.dma("sp" if t % 2 == 0 else "act", hu_d[t * 128:(t + 1) * 128, :], h_tm[:, t, :], f"hu{t}",
                              reads=[("h", t)], writes=[("hu", t)])
                w_release()
            dump("h1", h_tm, [128, NT, D], F32, R1_keys)

        if stop_after >= 5:
            P.fence(m_keys + ["junk", ("xn_bf", 0), ("xn_bf", 1), ("tn_f", 0), ("tn_f", 1), "tn_lo", "tlT"])
            P.dma("sp", gbc[:], gffn_d, "c_gbc", writes=["gbc"])
            Lg = rt[:, 0:160].rearrange("p (t c) -> p t c", c=20)
            rms_stats(lambda t: h_tm[:, t, :], 1, 0)
            for t in range(NT):
                if t + 1 < NT:
                    rms_stats(lambda t: h_tm[:, t, :], 1, t + 1)
                xb = xn_bf[t % 2]
                tnf = (tn_f, tn_f2)[t % 2]
                tnk = ("tn_f", t % 2)
                P.add("dve", lambda e, t=t, tnf=tnf: e.scalar_tensor_tensor(out=tnf, in0=h_tm[:, t, :], scalar=rstd[:, 1, t:t + 1], in1=gbc[:],
                                                                   op0=ALU.mult, op1=ALU.mult),
                      reads=[("h", t), ("rstd", 1, t), "gbc"], writes=[tnk])
                P.add("act", lambda e, xb=xb, tnf=tnf: e.copy(out=xb, in_=tnf), reads=[tnk], writes=[("xn_bf", t % 2)])
                P.dma("sp", tnu_d[t * 128:(t + 1) * 128, :], xb, f"tnu{t % 2}", reads=[("xn_bf", t % 2)], writes=[("tnu", t)])
                P.add("dve", lambda e, xb=xb, tnf=tnf: e.tensor_tensor(out=tn_lo, in0=tnf, in1=xb, op=ALU.subtract),
                      reads=[tnk, ("xn_bf", t % 2)], writes=["tn_lo"])
                transpose_tile(xb, xnT, t, ("xn_bf", t % 2), ("xnT", t), evac_alt=t)
                transpose_tile(tn_lo, tlT, 0, "tn_lo", "tlT", evac_alt=t)
                b = banks(1)

                def g(e, t=t, b=b):
                    ins = None
                    o = ps[:, b, 0:20]
                    n_mm = 3 * KC
                    i = 0
                    for (lt, wt) in ((0, wr_hi), (1, wr_hi), (0, wr_lo)):
                        for k in range(KC):
                            lhs = xnT[:, k, t * 128:(t + 1) * 128] if lt == 0 else tlT[:, k, :]
                            ins = e.matmul(o, lhsT=lhs, rhs=wt[:, k, :], start=(i == 0), stop=(i == n_mm - 1))
                            i += 1
                    return ins
                P.add("pe", g, reads=[("xnT", t), "tlT", "wr_hi", "wr_lo"], writes=[PS(b)])
                P.add("dve", lambda e, t=t, b=b: e.tensor_tensor(out=Lg[:, t, :], in0=ps[:, b, 0:20], in1=br[:], op=ALU.add),
                      reads=[PS(b), "br"], writes=["Lg"])
            dump("tT", xnT[:], [128, KC, TOK], BF16, xnT_keys)
            dump("Lg", rt[:, 0:160], [128, 160], F32, ["Lg"])

            o = [160]

            def tmp(n, c=None):
                a = rt[:, o[0]:o[0] + n]
                o[0] += n
                return a if c is None else a.rearrange("p (t c) -> p t c", c=c)
            lg = Lg[:, :, 0:4]
            le = Lg[:, :, 4:20]
            mg = tmp(8); ohg = tmp(32, 4); eg = tmp(32, 4); sumg = tmp(8); pg = tmp(8)
            selv = tmp(32, 4); tm4 = tmp(32, 4); m1 = tmp(8); mk1 = tmp(32, 4); sel2 = tmp(32, 4)
            m2 = tmp(8); mk2 = tmp(32, 4); dd = tmp(8); e2 = tmp(8); w1 = tmp(8); w2 = tmp(8)
            winn = tmp(32, 4); ogp = tmp(32, 4)
            bc = lambda a: a.unsqueeze(2).to_broadcast([128, NT, 4])
            R = ["Lg", "rt"]
            V = lambda fn: P.add("dve", fn, reads=R, writes=["rt"])
            V(lambda e: e.tensor_reduce(out=mg, in_=lg, axis=AX.X, op=ALU.max))
            V(lambda e: e.tensor_tensor(out=ohg, in0=lg, in1=bc(mg), op=ALU.is_equal))

        if stop_after >= 6:
            IOA = bass.IndirectOffsetOnAxis
            V(lambda e: e.tensor_copy(out=ohg_bf[:], in_=ohg))
            b = banks(1)
            ones_bf = tri[:, 0:128]
            U_bf = tri[:, 128:256]

            def g(e, b=b):
                ins = None
                for t in range(NT):
                    ins = e.matmul(ps[:, b, 0:4], lhsT=ones_bf, rhs=ohg_bf[:, t, :], start=(t == 0), stop=(t == NT - 1))
                for t in range(NT):
                    o = ps[:, b, 4 + 4 * t:8 + 4 * t]
                    for t2 in range(t):
                        ins = e.matmul(o, lhsT=ones_bf, rhs=ohg_bf[:, t2, :], start=(t2 == 0), stop=False)
                    ins = e.matmul(o, lhsT=U_bf, rhs=ohg_bf[:, t, :], start=(t == 0), stop=True)
                return ins
            P.add("pe", g, reads=["rt", "tri"], writes=[PS(b)])
            cnt = tmp(4); start = tmp(4); end = tmp(4); rk = tmp(32, 4); dest_f = tmp(8); Ff = tmp(32, 8); tm8 = tmp(8)
            Rb = ["rt", "Lg", PS(b), "thr"]
            Vb = lambda fn: P.add("dve", fn, reads=Rb, writes=["rt"])
            Vb(lambda e: e.tensor_copy(out=cnt, in_=ps[:, b, 0:4]))
            Vb(lambda e: e.memset(start[:, 0:1], 0.0))
            Vb(lambda e: e.tensor_copy(out=start[:, 1:2], in_=cnt[:, 0:1]))
            Vb(lambda e: e.tensor_tensor(out=start[:, 2:3], in0=start[:, 1:2], in1=cnt[:, 1:2], op=ALU.add))
            Vb(lambda e: e.tensor_tensor(out=start[:, 3:4], in0=start[:, 2:3], in1=cnt[:, 2:3], op=ALU.add))
            Vb(lambda e: e.tensor_tensor(out=end, in0=start, in1=cnt, op=ALU.add))
            Vb(lambda e: e.tensor_tensor(out=rk, in0=ps[:, b, 4:36].rearrange("p (t c) -> p t c", c=4),
                                         in1=start.unsqueeze(1).to_broadcast([128, NT, 4]), op=ALU.add))
            Vb(lambda e: e.tensor_tensor(out=rk, in0=rk, in1=ohg, op=ALU.mult))
            Vb(lambda e: e.tensor_reduce(out=dest_f, in_=rk, axis=AX.X, op=ALU.add))
            P.add("dve", lambda e: e.tensor_copy(out=dest_i[:], in_=dest_f), reads=["rt"], writes=["dest_i"])
            for gi in range(4):
                Vb(lambda e, gi=gi: e.tensor_scalar(out=Ff[:, gi, :], in0=thr[:, 8:16], scalar1=start[:, gi:gi + 1], scalar2=None, op0=ALU.is_gt))
                Vb(lambda e, gi=gi: e.tensor_scalar(out=tm8, in0=thr[:, 0:8], scalar1=end[:, gi:gi + 1], scalar2=None, op0=ALU.is_lt))
                Vb(lambda e, gi=gi: e.tensor_tensor(out=Ff[:, gi, :], in0=Ff[:, gi, :], in1=tm8, op=ALU.mult))
            P.add("dve", lambda e: e.tensor_copy(out=F_i[:], in_=Ff.rearrange("p g j -> p (g j)")), reads=["rt"], writes=["flags"])
            dump("dest", dest_i[:], [128, NT], mybir.dt.int32, ["dest_i"])
            dump("flags", F_i[:], [128, 32], mybir.dt.int32, ["flags"])
            for t in range(NT):
                P.dma("pool", iv_d, tokid[:, t, :], "iv", indirect=True, out_offset=IOA(ap=dest_i[:, t:t + 1], axis=0), in_offset=None,
                      reads=["dest_i", "tokid"], writes=[("iv_d", t)])
            P.dma("sp", inv_sb[:], iv_d.rearrange("(j p) c -> p j c", p=128), "ivl", reads=[("iv_d", t) for t in range(NT)], writes=["inv"])
            dump("inv", inv_sb[:], [128, NT, 16], mybir.dt.int32, ["inv"])
            V(lambda e: e.tensor_tensor(out=eg, in0=lg, in1=bc(mg), op=ALU.subtract))
            P.add("act", lambda e: e.activation(out=eg, in_=eg, func=AF.Exp), reads=R, writes=["rt"])
            V(lambda e: e.tensor_reduce(out=sumg, in_=eg, axis=AX.X, op=ALU.add))
            V(lambda e: e.reciprocal(out=pg, in_=sumg))
            for gi in range(4):
                if gi == 0:
                    V(lambda e: e.tensor_tensor(out=selv, in0=le[:, :, 0:4], in1=bc(ohg[:, :, 0]), op=ALU.mult))
                else:
                    V(lambda e, gi=gi: e.tensor_tensor(out=tm4, in0=le[:, :, 4 * gi:4 * gi + 4], in1=bc(ohg[:, :, gi]), op=ALU.mult))
                    V(lambda e: e.tensor_tensor(out=selv, in0=selv, in1=tm4, op=ALU.add))
            V(lambda e: e.tensor_reduce(out=m1, in_=selv, axis=AX.X, op=ALU.max))
            V(lambda e: e.tensor_tensor(out=mk1, in0=selv, in1=bc(m1), op=ALU.is_equal))
            V(lambda e: e.scalar_tensor_tensor(out=sel2, in0=mk1, scalar=-1e30, in1=selv, op0=ALU.mult, op1=ALU.add))
            V(lambda e: e.tensor_reduce(out=m2, in_=sel2, axis=AX.X, op=ALU.max))
            V(lambda e: e.tensor_tensor(out=mk2, in0=sel2, in1=bc(m2), op=ALU.is_equal))
            V(lambda e: e.tensor_tensor(out=dd, in0=m2, in1=m1, op=ALU.subtract))
            P.add("act", lambda e: e.activation(out=e2, in_=dd, func=AF.Exp), reads=R, writes=["rt"])
            V(lambda e: e.tensor_scalar(out=w1, in0=e2, scalar1=1.0, scalar2=None, op0=ALU.add))
            V(lambda e: e.reciprocal(out=w1, in_=w1))
            V(lambda e: e.tensor_tensor(out=w2, in0=e2, in1=w1, op=ALU.mult))
            V(lambda e: e.tensor_tensor(out=winn, in0=mk1, in1=bc(w1), op=ALU.mult))
            V(lambda e: e.tensor_tensor(out=tm4, in0=mk2, in1=bc(w2), op=ALU.mult))
            V(lambda e: e.tensor_tensor(out=winn, in0=winn, in1=tm4, op=ALU.add))
            V(lambda e: e.tensor_tensor(out=ogp, in0=ohg, in1=bc(pg), op=ALU.mult))
            for gi in range(4):
                P.add("dve", lambda e, gi=gi: e.tensor_tensor(out=gate[:, :, 4 * gi:4 * gi + 4], in0=winn, in1=bc(ogp[:, :, gi]), op=ALU.mult),
                      reads=R, writes=["gate"])
            dump("gate", gate[:], [128, NT, NE], F32, ["gate"])
            P.dma("sp", gu_d.rearrange("(t p) c -> p t c", p=128), gate[:], "gu", reads=["gate"], writes=["gu"])
            P.fence(["junk", ("tn_f", 0), ("tn_f", 1), "tn_lo", "tlT"])
            P.fence(["tn_lo", "tlT", ("tn_f", 1), ("w", 3)] + [("m", d) for d in range(KC)] + [("cz", j) for j in range(4)] + [("acc", j) for j in range(4)])
            P.fence(["gbc", "wsT", "wsT_f", "sel", ("w", 4)])
            enable_slot3()
            for j in range(NT):
                P.dma("pool", xn_bf[j % 2], tnu_d, f"gt{j % 2}", indirect=True, out_offset=None, in_offset=IOA(ap=inv_sb[:, j, 0:1], axis=0),
                      reads=["inv"] + [("tnu", t) for t in range(NT)], writes=[("xn_bf", j % 2)])
                transpose_tile(xn_bf[j % 2], xnT, j, ("xn_bf", j % 2), ("xnT", j), evac_alt=j)
            for j in range(NT):
                P.dma("pool", gate_s[:, j, :], gu_d, f"gg{j}", indirect=True, out_offset=None, in_offset=IOA(ap=inv_sb[:, j, 0:1], axis=0),
                      reads=["inv", "gu"], writes=[("gate_s", j)])
            for j in range(NT):
                P.dma("pool", h_tm[:, j, :], hu_d, f"gh{j}", indirect=True, out_offset=None, in_offset=IOA(ap=inv_sb[:, j, 0:1], axis=0),
                      reads=["inv"] + [("hu", t) for t in range(NT)], writes=[("h", j)])
            dump("tTs", xnT[:], [128, KC, TOK], BF16, xnT_keys)
            dump("gate_s", gate_s[:], [128, NT, NE], F32, [("gate_s", j) for j in range(NT)])

        if stop_after >= 6:
            P.fence(["junk", ("xn_bf", 0), ("xn_bf", 1), ("tn_f", 0), ("tn_f", 1), "tn_lo", "tlT", ("sg", 0), ("sg", 1)]
                    + [("hid", j) for j in range(NT)] + [("hid_tm", j) for j in range(NT)])
            ENG3 = ("pe", "act", "dve")
            for ex in range(NE):
                gi = ex // 4
                if ex % 4 == 0:
                    for en in ENG3:
                        def ld(engine, en=en, gi=gi):
                            if en not in P.regs:
                                P.regs[en] = [engine.alloc_register(f"flag_{en}_{j}") for j in range(NT)]
                            ins = None
                            for j in range(NT):
                                ins = engine.reg_load(P.regs[en][j], F_i[0:1, gi * 8 + j:gi * 8 + j + 1])
                            return ins
                        P.add(en, ld, reads=["flags"])
                wg_v, kg = w_acquire()
                wu_v, ku = w_acquire()
                hT = hidT1
                hk = "hid"
                for j in range(NT):
                    P.cond_begin(ENG3, j)
                    bg = banks(1)
                    bu = banks(1)
                    def g(e, j=j, bg=bg, bu=bu, wg_v=wg_v, wu_v=wu_v):
                        ins = None
                        for k in range(KC):
                            lhs = xnT[:, k, j * 128:(j + 1) * 128]
                            e.matmul(ps[:, bg, :], lhsT=lhs, rhs=wg_v[:, k, :], start=(k == 0), stop=(k == KC - 1))
                            ins = e.matmul(ps[:, bu, :], lhsT=lhs, rhs=wu_v[:, k, :], start=(k == 0), stop=(k == KC - 1))
                        return ins
                    P.add("pe", g, reads=[kg, ku, ("xnT", j)], writes=[PS(bg), PS(bu)])
                    P.add("act", lambda e, j=j, bg=bg: e.activation(out=sg[j % 2], in_=ps[:, bg, :], func=AF.Silu),
                          reads=[PS(bg)], writes=[("sg", j % 2)])
                    P.add("dve", lambda e, j=j, bu=bu, ex=ex: e.scalar_tensor_tensor(out=hid_tm[:, j, :], in0=ps[:, bu, :], scalar=gate_s[:, j, ex:ex + 1],
                                                                                   in1=sg[j % 2], op0=ALU.mult, op1=ALU.mult),
                          reads=[PS(bu), ("sg", j % 2), ("gate_s", j)], writes=[("hid_tm", j)])
                    P.cond_end(ENG3)
                w_release()
                w_release()
                for j in range(NT):
                    P.cond_begin(("pe", "act"), j)
                    bt = banks(1)
                    pst = ps[:, bt, :].bitcast(BF16)

                    def trg(e, j=j, pst=pst):
                        ins = None
                        for k in range(4):
                            ins = e.transpose(out=pst[:, k * 128:(k + 1) * 128], in_=hid_tm[:, j, k * 128:(k + 1) * 128], identity=ident[:])
                        return ins
                    P.add("pe", trg, reads=[("hid_tm", j), "ident"], writes=[PS(bt)])
                    P.add("act", lambda e, j=j, pst=pst, hT=hT: e.copy(out=hT[:, 0:4, j * 128:(j + 1) * 128],
                                                                       in_=pst[:, 0:512].rearrange("p (k c) -> p k c", c=128)),
                          reads=[PS(bt)], writes=[("hid", j)])
                    P.cond_end(("pe", "act"))
                wd_v, kd = w_acquire()
                for j in range(NT):
                    P.cond_begin(("pe", "dve"), j)
                    b0 = banks(4)

                    def g(e, j=j, b0=b0, hT=hT, wd_v=wd_v):
                        ins = None
                        for k in range(4):
                            lhs = hT[:, k, j * 128:(j + 1) * 128]
                            for n in range(4):
                                ins = e.matmul(ps[:, b0 + n, :], lhsT=lhs, rhs=wd_v[:, k, n * 512:(n + 1) * 512], start=(k == 0), stop=(k == 3))
                        return ins
                    P.add("pe", g, reads=[kd, ("hid", j)], writes=[PS(b0 + i) for i in range(4)])
                    P.add("dve", lambda e, j=j, b0=b0: e.tensor_tensor(
                        out=h_tm[:, j, :], in0=ps[:, b0:b0 + 4, :].rearrange("p a b -> p (a b)"), in1=h_tm[:, j, :], op=ALU.add),
                        reads=[PS(b0 + i) for i in range(4)] + [("h", j)], writes=[("h", j)])
                    P.cond_end(("pe", "dve"))
                w_release()
            dump("h2s", h_tm, [128, NT, D], F32, R1_keys)

        if stop_after >= 7:
            P.fence([("w", 4), "gbc"])
            P.dma("sp", gbc[:], gfin_d, "c_gbc", writes=["gbc"])
            P.fence(["junk", ("sg", 0), ("sg", 1)] + [("hid", j) for j in range(NT)] + [("hid_tm", j) for j in range(NT)])
            rms_stats(lambda t: h_tm[:, t, :], 2, 0)
            for t in range(NT):
                if t + 1 < NT:
                    rms_stats(lambda t: h_tm[:, t, :], 2, t + 1)
                P.add("dve", lambda e, t=t: e.scalar_tensor_tensor(out=h_tm[:, t, :], in0=h_tm[:, t, :], scalar=rstd[:, 2, t:t + 1], in1=gbc[:],
                                                                   op0=ALU.mult, op1=ALU.mult),
                      reads=[("h", t), ("rstd", 2, t), "gbc"], writes=[("h", t)])
                P.dma("pool", out_d, h_tm[:, t, :], f"o{t}", indirect=True, out_offset=IOA(ap=inv_sb[:, t, 0:1], axis=0), in_offset=None,
                      reads=[("h", t), "inv"], writes=[("out", t)])
        P.add("sp", None, reads=[("out", t) for t in range(NT)] + [("dbg", n) for n in dbg_out])
        P.emit()
    return nc, dbg_out


def make_in_maps(x, norm_mix_g, w_in, conv_w, sgu_ln_g, sgu_ln_b, sgu_w_s, sgu_b_s,
                 w_up_conv, w_up_sgu, w_out, norm_ffn_g, w_router_group, b_router_group,
                 w_router_expert, b_router_expert, w_exp_gate, w_exp_up, w_exp_down, norm_final_g, ncores=NCORES):
    f = lambda a: np.ascontiguousarray(np.asarray(a, dtype=np.float32))
    x2 = f(x).reshape(SEQ, D)
    bcast = lambda v, n: np.ascontiguousarray(np.broadcast_to(f(v).reshape(1, n), (128, n)))
    wr = np.concatenate([f(w_router_group)[0], f(w_router_expert)[0]], axis=1)
    br = np.concatenate([f(b_router_group)[0], f(b_router_expert)[0]], axis=0)
    sel = np.zeros((8, 1024), dtype=ml_dtypes.bfloat16)
    for h in range(8):
        sel[h, h * 128:(h + 1) * 128] = 1
    shared = {
        "w_in": f(w_in)[0], "w_up_conv": f(w_up_conv)[0], "w_up_sgu": f(w_up_sgu)[0], "w_out": f(w_out)[0],
        "w_exp_gate": f(w_exp_gate)[0], "w_exp_up": f(w_exp_up)[0], "w_exp_down": f(w_exp_down)[0],
        "gmix_bc": bcast(norm_mix_g, D), "gffn_bc": bcast(norm_ffn_g, D), "gfin_bc": bcast(norm_final_g, D),
        "lng_bc": bcast(sgu_ln_g, SW), "lnb_bc": bcast(sgu_ln_b, SW),
        "cw": np.ascontiguousarray(f(conv_w)[0].reshape(3, 8, 128).transpose(2, 1, 0)),
        "wsT": np.ascontiguousarray(f(sgu_w_s)[0].transpose(2, 0, 1)),
        "bs": f(sgu_b_s)[0],
        "wr": np.ascontiguousarray(wr.reshape(KC, 128, 20).transpose(1, 0, 2)),
        "br_bc": bcast(br, 20),
        "ident": np.eye(128, dtype=ml_dtypes.bfloat16),
        "sel": sel,
        "tri": np.concatenate([np.ones((128, 128), np.float32), np.triu(np.ones((128, 128), np.float32), 1)], axis=1).astype(ml_dtypes.bfloat16),
        "thr": np.ascontiguousarray(np.broadcast_to(np.concatenate([128.0 * np.arange(8), 128.0 * (np.arange(8) + 1)]).astype(np.float32), (128, 16))),
        "tokid": np.ascontiguousarray(np.broadcast_to((np.arange(NT)[None, :, None] * 128 + np.arange(128)[:, None, None]).astype(np.int32), (128, NT, 16))),
    }
    maps = []
    for c in range(ncores):
        m = dict(shared)
        m["x"] = np.ascontiguousarray(x2[c * TOK:(c + 1) * TOK])
        m["xh"] = np.ascontiguousarray(x2[c * TOK - 2:c * TOK]) if c > 0 else np.zeros((2, D), np.float32)
        maps.append(m)
    return maps


_NC_CACHE = {}


def kernel(**inputs):
    if "nc" not in _NC_CACHE:
        _NC_CACHE["nc"] = build_program()[0]
    nc = _NC_CACHE["nc"]
    in_maps = make_in_maps(**inputs)
    res = run_bass_kernel_spmd(nc, in_maps, core_ids=list(range(NCORES)))
    out = np.concatenate([np.asarray(r["out"]) for r in res.results], axis=0)
    return out.reshape(1, SEQ, D).astype(np.float32)
```
